# Optimizing a Trainium2 kernel written in Bass

```python
import jax, jax.numpy as jnp
from jax import lax
import numpy as np

D_MODEL = 4096
BATCH = 4
SEQ = 4096
DEPTH = 2

BLOCK_Q = 128
N_BRANCH = 4
BRANCH_WIDTH = 1024
A_HEADS = 8
A_KV_HEADS = 2
A_DIM = 128
IDX_HEADS = 16
IDX_DIM = 64
IDX_TOPK_MAX = 256
B_HEADS = 8
B_DIM = 128
C_HEADS = 16
C_KV_HEADS = 2
C_DIM = 64
WINDOW = 128
D_HEADS = 8
D_Q_LORA = 768
D_KV_LORA = 256
D_NOPE = 128
D_ROPE = 64
D_VDIM = 128
ROPE_THETA = 10000.0
RMS_EPS = 1e-6
LN_EPS = 1e-5
DEEPNORM_ALPHA = (2 * DEPTH) ** 0.25
DEEPNORM_BETA = (8 * DEPTH) ** -0.25
MAX_START_POS = 1024

IN_SEGMENTS = (
    ('a_q', A_HEADS * A_DIM), ('a_k', A_KV_HEADS * A_DIM), ('a_v', A_KV_HEADS * A_DIM),
    ('a_iq', IDX_HEADS * IDX_DIM), ('a_ik', IDX_DIM), ('a_iw', IDX_HEADS), ('a_z', BRANCH_WIDTH),
    ('b_q', B_HEADS * B_DIM), ('b_k', B_HEADS * B_DIM), ('b_v', B_HEADS * B_DIM), ('b_f', B_HEADS), ('b_z', BRANCH_WIDTH),
    ('c_q', C_HEADS * C_DIM), ('c_k', C_KV_HEADS * C_DIM), ('c_v', C_KV_HEADS * C_DIM), ('c_z', BRANCH_WIDTH),
    ('d_cq', D_Q_LORA), ('d_ckv', D_KV_LORA), ('d_kr', D_ROPE), ('d_z', BRANCH_WIDTH),
)
IN_WIDTH = sum(n for _, n in IN_SEGMENTS)

kernel_name = 'hybrid_gated_four_mixer_deepnorm'


def split_columns(h):
    sizes = [n for _, n in IN_SEGMENTS]
    offsets = [int(o) for o in np.cumsum(sizes)[:-1]]
    return dict(zip([name for name, _ in IN_SEGMENTS], jnp.split(h, offsets, axis=-1)))


def alibi_slopes(n_heads):
    return jnp.asarray([2.0 ** (-8.0 * (h + 1) / n_heads) for h in range(n_heads)], dtype=jnp.float32)


def to_blocks(t, nb):
    return jnp.moveaxis(t.reshape(t.shape[0], nb, BLOCK_Q, *t.shape[2:]), 1, 0)


def from_blocks(t):
    t = jnp.moveaxis(t, 0, 1)
    return t.reshape(t.shape[0], t.shape[1] * t.shape[2], -1)


def rms_norm(x, g):
    xf = x.astype(jnp.float32)
    return (xf * lax.rsqrt(jnp.mean(jnp.square(xf), -1, keepdims=True) + RMS_EPS) * g).astype(x.dtype)


def layer_norm(x, g, b):
    xf = x.astype(jnp.float32)
    mu = jnp.mean(xf, -1, keepdims=True)
    var = jnp.mean(jnp.square(xf - mu), -1, keepdims=True)
    return ((xf - mu) * lax.rsqrt(var + LN_EPS) * g + b).astype(x.dtype)


def rope(t, positions):
    half = t.shape[-1] // 2
    inv_freq = ROPE_THETA ** (-jnp.arange(half, dtype=jnp.float32) / half)
    ang = positions.astype(jnp.float32)[..., None] * inv_freq
    cos = jnp.cos(ang)[:, :, None, :]
    sin = jnp.sin(ang)[:, :, None, :]
    t1 = t[..., :half].astype(jnp.float32)
    t2 = t[..., half:].astype(jnp.float32)
    return jnp.concatenate([t1 * cos - t2 * sin, t1 * sin + t2 * cos], -1).astype(t.dtype)


def indexed_sparse_attention(q, k, v, iq, ik, iw):
    B, S = q.shape[:2]
    nb = S // BLOCK_Q
    topk = min(IDX_TOPK_MAX, S // 4)
    slopes = alibi_slopes(A_HEADS).reshape(A_KV_HEADS, A_HEADS // A_KV_HEADS)
    scale = A_DIM ** -0.5
    w = iw.astype(jnp.float32) * (IDX_HEADS ** -0.5 * IDX_DIM ** -0.5)
    kpos = jnp.arange(S)
    gather = jax.vmap(lambda arr, idx: arr[idx])

    def block(args):
        i, q_i, iq_i, w_i = args
        qpos = i * BLOCK_Q + jnp.arange(BLOCK_Q)
        causal = kpos[None, :] <= qpos[:, None]
        rel = jax.nn.relu(jnp.einsum('bqhd,bsd->bqhs', iq_i, ik).astype(jnp.float32))
        score = jnp.einsum('bqhs,bqh->bqs', rel, w_i)
        score = jnp.where(causal[None], score, -jnp.inf)
        top_val, top_idx = lax.top_k(score, topk)
        valid = jnp.isfinite(top_val)
        k_sel = gather(k, top_idx)
        v_sel = gather(v, top_idx)
        logits = jnp.einsum('bqgrd,bqkgd->bgrqk', q_i, k_sel).astype(jnp.float32) * scale
        dist = (qpos[None, :, None] - top_idx).astype(jnp.float32)
        logits = logits - slopes[None, :, :, None, None] * dist[:, None, None]
        logits = jnp.where(valid[:, None, None], logits, -jnp.inf)
        p = jax.nn.softmax(logits, axis=-1)
        return jnp.einsum('bgrqk,bqkgd->bqgrd', p.astype(v.dtype), v_sel)

    out = lax.map(block, (jnp.arange(nb), to_blocks(q, nb), to_blocks(iq, nb), to_blocks(w, nb)))
    return from_blocks(out)


def blocked_causal_attention(q, k, v, scale, cum_log_forget=None):
    B, S = q.shape[:2]
    nb = S // BLOCK_Q
    kpos = jnp.arange(S)
    xs = (jnp.arange(nb), to_blocks(q, nb))
    if cum_log_forget is not None:
        c_keys = jnp.transpose(cum_log_forget, (0, 2, 1))
        xs = xs + (to_blocks(cum_log_forget, nb),)

    def block(args):
        i, q_i = args[0], args[1]
        qpos = i * BLOCK_Q + jnp.arange(BLOCK_Q)
        logits = jnp.einsum('bqhd,bkhd->bhqk', q_i, k).astype(jnp.float32) * scale
        if cum_log_forget is not None:
            c_q = jnp.transpose(args[2], (0, 2, 1))
            logits = logits + (c_q[..., None] - c_keys[:, :, None, :])
        causal = kpos[None, :] <= qpos[:, None]
        logits = jnp.where(causal[None, None], logits, -jnp.inf)
        p = jax.nn.softmax(logits, axis=-1)
        return jnp.einsum('bhqk,bkhd->bqhd', p.astype(v.dtype), v)

    return from_blocks(lax.map(block, xs))


def sliding_window_sink_attention(q, k, v, sinks):
    B, S = q.shape[:2]
    nb = S // WINDOW
    G, R = C_KV_HEADS, C_HEADS // C_KV_HEADS
    slopes = alibi_slopes(C_HEADS).reshape(G, R)
    qb = q.reshape(B, nb, WINDOW, G, R, C_DIM)
    kb = k.reshape(B, nb, WINDOW, G, C_DIM)
    vb = v.reshape(B, nb, WINDOW, G, C_DIM)
    shift = lambda t: jnp.pad(t, ((0, 0), (1, 0), (0, 0), (0, 0), (0, 0)))[:, :-1]
    kcat = jnp.concatenate([shift(kb), kb], axis=2)
    vcat = jnp.concatenate([shift(vb), vb], axis=2)
    logits = jnp.einsum('bnqgrd,bnkgd->bngrqk', qb, kcat).astype(jnp.float32) * (C_DIM ** -0.5)
    qi = jnp.arange(WINDOW)[:, None]
    kj = jnp.arange(2 * WINDOW)[None, :]
    dist = qi + WINDOW - kj
    band = (dist >= 0) & (dist < WINDOW)
    has_prev = (jnp.arange(nb) > 0)[:, None, None] | (kj >= WINDOW)[None]
    mask = band[None] & has_prev
    logits = logits - slopes[None, None, :, :, None, None] * dist.astype(jnp.float32)
    logits = jnp.where(mask[None, :, None, None], logits, -jnp.inf)
    sink = sinks.astype(jnp.float32).reshape(G, R)[None, None, :, :, None]
    lse = jnp.logaddexp(jax.nn.logsumexp(logits, axis=-1), sink)
    p = jnp.exp(logits - lse[..., None])
    out = jnp.einsum('bngrqk,bnkgd->bnqgrd', p.astype(v.dtype), vcat)
    return out.reshape(B, S, C_HEADS * C_DIM)


def latent_attention(c_q, c_kv, k_rope_in, positions, q_gain, q_up, kv_gain, kv_up):
    B, S = c_q.shape[:2]
    q = jnp.einsum('bsc,ce->bse', rms_norm(c_q, q_gain), q_up).reshape(B, S, D_HEADS, D_NOPE + D_ROPE)
    q = jnp.concatenate([q[..., :D_NOPE], rope(q[..., D_NOPE:], positions)], -1)
    kv = jnp.einsum('bsc,ce->bse', rms_norm(c_kv, kv_gain), kv_up).reshape(B, S, D_HEADS, D_NOPE + D_VDIM)
    k_rope = rope(k_rope_in[:, :, None, :], positions)
    k = jnp.concatenate([kv[..., :D_NOPE], jnp.broadcast_to(k_rope, (B, S, D_HEADS, D_ROPE))], -1)
    v = kv[..., D_NOPE:]
    return blocked_causal_attention(q, k, v, (D_NOPE + D_ROPE) ** -0.5)


def hybrid_layer(x, positions, w_in, w_gate, w_branch, w_out, dq_gain, dq_up, dkv_gain, dkv_up, f_bias, sinks, ln_gain, ln_bias):
    B, S, _ = x.shape
    p = split_columns(jnp.einsum('bsd,de->bse', x, w_in))
    o_a = indexed_sparse_attention(
        p['a_q'].reshape(B, S, A_KV_HEADS, A_HEADS // A_KV_HEADS, A_DIM),
        p['a_k'].reshape(B, S, A_KV_HEADS, A_DIM), p['a_v'].reshape(B, S, A_KV_HEADS, A_DIM),
        p['a_iq'].reshape(B, S, IDX_HEADS, IDX_DIM), p['a_ik'], p['a_iw'])
    log_f = jax.nn.log_sigmoid(p['b_f'].astype(jnp.float32) + f_bias.astype(jnp.float32))
    o_b = blocked_causal_attention(
        p['b_q'].reshape(B, S, B_HEADS, B_DIM), p['b_k'].reshape(B, S, B_HEADS, B_DIM),
        p['b_v'].reshape(B, S, B_HEADS, B_DIM), B_DIM ** -0.5, cum_log_forget=jnp.cumsum(log_f, axis=1))
    o_c = sliding_window_sink_attention(
        p['c_q'].reshape(B, S, C_KV_HEADS, C_HEADS // C_KV_HEADS, C_DIM),
        p['c_k'].reshape(B, S, C_KV_HEADS, C_DIM), p['c_v'].reshape(B, S, C_KV_HEADS, C_DIM), sinks)
    o_d = latent_attention(p['d_cq'], p['d_ckv'], p['d_kr'], positions, dq_gain, dq_up, dkv_gain, dkv_up)
    branches = (o_a * jax.nn.silu(p['a_z']), o_b * jax.nn.silu(p['b_z']),
                o_c * jax.nn.silu(p['c_z']), o_d * jax.nn.silu(p['d_z']))
    merged = jnp.zeros_like(x)
    for n in range(N_BRANCH):
        gate = jax.nn.sigmoid(jnp.einsum('bsd,de->bse', x, w_gate[n]))
        merged = merged + gate * jnp.einsum('bsc,cd->bsd', branches[n], w_branch[n])
    y = jnp.einsum('bsd,de->bse', merged, w_out)
    return layer_norm(DEEPNORM_ALPHA * x + y, ln_gain, ln_bias)


def setup_inputs(seed: int = 0) -> dict:
    key = jax.random.key(seed)
    ks = jax.random.split(key, 16)
    nrm = lambda k, shape, s: jax.random.normal(k, shape, jnp.float32) * s
    x = nrm(ks[0], (BATCH, SEQ, D_MODEL), 1.0)
    start = jax.random.randint(ks[1], (BATCH, 1), 0, MAX_START_POS, dtype=jnp.int32)
    positions = start + jnp.arange(SEQ, dtype=jnp.int32)[None, :]
    w_in = nrm(ks[2], (DEPTH, D_MODEL, IN_WIDTH), D_MODEL ** -0.5)
    w_gate = nrm(ks[3], (DEPTH, N_BRANCH, D_MODEL, D_MODEL), D_MODEL ** -0.5)
    w_branch = nrm(ks[4], (DEPTH, N_BRANCH, BRANCH_WIDTH, D_MODEL), BRANCH_WIDTH ** -0.5 * DEEPNORM_BETA)
    w_out = nrm(ks[5], (DEPTH, D_MODEL, D_MODEL), D_MODEL ** -0.5 * DEEPNORM_BETA)
    dq_gain = 1.0 + nrm(ks[6], (DEPTH, D_Q_LORA), 0.02)
    dq_up = nrm(ks[7], (DEPTH, D_Q_LORA, D_HEADS * (D_NOPE + D_ROPE)), D_Q_LORA ** -0.5)
    dkv_gain = 1.0 + nrm(ks[8], (DEPTH, D_KV_LORA), 0.02)
    dkv_up = nrm(ks[9], (DEPTH, D_KV_LORA, D_HEADS * (D_NOPE + D_VDIM)), D_KV_LORA ** -0.5)
    f_bias = jax.random.uniform(ks[10], (DEPTH, B_HEADS), jnp.float32, 1.0, 5.0)
    sinks = nrm(ks[11], (DEPTH, C_HEADS), 0.5)
    ln_gain = 1.0 + nrm(ks[12], (DEPTH, D_MODEL), 0.02)
    ln_bias = nrm(ks[13], (DEPTH, D_MODEL), 0.02)
    return {'x': x, 'positions': positions, 'w_in': w_in, 'w_gate': w_gate, 'w_branch': w_branch,
            'w_out': w_out, 'dq_gain': dq_gain, 'dq_up': dq_up, 'dkv_gain': dkv_gain, 'dkv_up': dkv_up,
            'f_bias': f_bias, 'sinks': sinks, 'ln_gain': ln_gain, 'ln_bias': ln_bias}


def reference(x, positions, w_in, w_gate, w_branch, w_out, dq_gain, dq_up, dkv_gain, dkv_up, f_bias, sinks, ln_gain, ln_bias):
    for l in range(DEPTH):
        x = hybrid_layer(x, positions, w_in[l], w_gate[l], w_branch[l], w_out[l], dq_gain[l], dq_up[l],
                         dkv_gain[l], dkv_up[l], f_bias[l], sinks[l], ln_gain[l], ln_bias[l])
    return x
```

```python
import math
from contextlib import ExitStack

import numpy as np
import concourse.bass as bass
import concourse.mybir as mybir
from concourse.bass_utils import run_bass_kernel_spmd

F32 = mybir.dt.float32
BF16 = mybir.dt.bfloat16
I32 = mybir.dt.int32
AF = mybir.ActivationFunctionType
ALU = mybir.AluOpType

D = 4096
KC = D // 128
DEPTH = 2
BATCH = 4
SEQ = 4096
INW = 12184
RMS_EPS = 1e-6
LN_EPS = 1e-5
ALPHA = (2 * DEPTH) ** 0.25
ROPE_THETA = 10000.0
NEG = -1.0e30

SEG = {}
_o = 0
for _n, _w in (('a_q', 1024), ('a_k', 256), ('a_v', 256), ('a_iq', 1024), ('a_ik', 64), ('a_iw', 16),
               ('a_z', 1024), ('b_q', 1024), ('b_k', 1024), ('b_v', 1024), ('b_f', 8), ('b_z', 1024),
               ('c_q', 1024), ('c_k', 128), ('c_v', 128), ('c_z', 1024), ('d_cq', 768), ('d_ckv', 256),
               ('d_kr', 64), ('d_z', 1024)):
    SEG[_n] = _o
    _o += _w
assert _o == INW


class Buf:
    __slots__ = ("w", "r")

    def __init__(self):
        self.w = []
        self.r = {}


class SemC:
    __slots__ = ("sem", "cnt")

    def __init__(self, sem):
        self.sem = sem
        self.cnt = 0


class Eng:
    def __init__(self, name, h, semc):
        self.name = name
        self.h = h
        self.semc = semc
        self.seen = {}
        self.dma_sems = []
        self.dma_i = 0


class Prog:
    def __init__(self, nc, ndma=8):
        self.nc = nc
        self.stack = ExitStack()

        def mk(name, h):
            s = self.stack.enter_context(nc.semaphore("s_" + name))
            return Eng(name, h, SemC(s))

        self.pe = mk("pe", nc.tensor)
        self.act = mk("act", nc.scalar)
        self.dve = mk("dve", nc.vector)
        self.pool = mk("pool", nc.gpsimd)
        self.sp = mk("sp", nc.sync)
        self.engs = [self.pe, self.act, self.dve, self.pool, self.sp]
        self.all_dma = []
        for e in (self.sp, self.pool, self.act):
            for i in range(ndma):
                s = self.stack.enter_context(nc.semaphore(f"d_{e.name}{i}"))
                sc = SemC(s)
                e.dma_sems.append(sc)
                self.all_dma.append(sc)
        self.nops = 0
        self.trace = {e.name: [] for e in self.engs}

    def _wait(self, eng, deps):
        best = {}
        for (sc, v) in deps:
            if best.get(sc, 0) < v:
                best[sc] = v
        for sc, v in best.items():
            if eng.seen.get(sc, 0) >= v:
                continue
            if sc is eng.semc and (eng is self.pe or v > sc.cnt):
                continue
            eng.h.wait_ge(sc.sem, v)
            self.trace[eng.name].append(('w', id(sc), v))
            eng.seen[sc] = v

    @staticmethod
    def _deps(reads, writes, join=False):
        deps = []
        for b in reads:
            deps.extend(b.w)
        for b in writes:
            if not join:
                deps.extend(b.w)
            deps.extend(b.r.items())
        return deps

    @staticmethod
    def _record(me, reads, writes, join=False):
        sc, v = me
        for b in reads:
            if b.r.get(sc, 0) < v:
                b.r[sc] = v
        for b in writes:
            if join:
                b.w.append(me)
            else:
                b.w = [me]
                b.r = {}

    def op(self, eng, fn, reads=(), writes=(), inc=True, join=False):
        self._wait(eng, self._deps(reads, writes, join))
        ins = fn(eng.h)
        self.nops += 1
        sc = eng.semc
        if inc:
            sc.cnt += 1
            ins.then_inc(sc.sem, 1)
            self.trace[eng.name].append(('i', id(sc), 1))
            me = (sc, sc.cnt)
        else:
            me = (sc, sc.cnt + 1)
        self._record(me, reads, writes, join)
        return ins

    def dma(self, eng, out, in_, reads=(), writes=(), join=False, **kw):
        sc = eng.dma_sems[eng.dma_i % len(eng.dma_sems)]
        eng.dma_i += 1
        deps = self._deps(reads, writes, join)
        if sc.cnt > 0:
            deps.append((sc, sc.cnt))
        self._wait(eng, deps)
        ins = eng.h.dma_start(out=out, in_=in_, **kw)
        sc.cnt += 16
        ins.then_inc(sc.sem, 16)
        self.trace[eng.name].append(('i', id(sc), 16))
        self.nops += 1
        self._record((sc, sc.cnt), reads, writes, join)
        return ins

    def barrier(self):
        deps = [(e.semc, e.semc.cnt) for e in self.engs if e.semc.cnt > 0]
        deps += [(sc, sc.cnt) for sc in self.all_dma if sc.cnt > 0]
        for e in self.engs:
            self._wait(e, [d for d in deps if d[0] is not e.semc or e is not self.pe])

    def close(self):
        self.stack.close()


def build_program(S, depth, dbg=(), upto=99):
    NB = S // 128
    NCH = S // 512
    TOPK = min(256, S // 4)
    nc = bass.Bass("TRN2", target_bir_lowering=False)
    P = Prog(nc)
    pe, act, dve, pool, sp = P.pe, P.act, P.dve, P.pool, P.sp
    gstack = ExitStack()

    def din(name, shape, dt=F32):
        return nc.dram_tensor(name, list(shape), dt, kind="ExternalInput").ap()

    x_in = din("x", [S, D])
    pos_in = din("positions", [1, S], I32)
    w_in = din("w_in", [depth, D, INW])
    w_gate = din("w_gate", [depth, 4, D, D])
    w_branch = din("w_branch", [depth, 4, 1024, D])
    w_out = din("w_out", [depth, D, D])
    dq_gain = din("dq_gain", [depth, 768])
    dq_up = din("dq_up", [depth, 768, 1536])
    dkv_gain = din("dkv_gain", [depth, 256])
    dkv_up = din("dkv_up", [depth, 256, 2048])
    f_bias = din("f_bias", [depth, 8])
    sinks = din("sinks", [depth, 16])
    ln_gain = din("ln_gain", [depth, D])
    ln_bias = din("ln_bias", [depth, D])
    consts = din("consts", [128, 4])
    out_t = nc.dram_tensor("out", [S, D], F32, kind="ExternalOutput").ap()

    def scr(name, shape, dt=BF16):
        return nc.dram_tensor(name, list(shape), dt, kind=("ExternalOutput" if name in dbg else "Internal")).ap()

    NSG = 26
    WIN = scr("WIN", [NSG, 128, KC, 512])
    WG = scr("WG", [32, 128, KC, 512])
    WB = scr("WB", [32, 128, 8, 512])
    WO = scr("WO", [8, 128, KC, 512])
    XT = scr("XT", [128, KC, S])
    X1 = out_t
    FM = {}
    for nm, g in (("QTa", 8), ("KTa", 2), ("IKT", 1), ("IQT", 8), ("QTb", 8), ("KTb", 8), ("QTc", 8),
                  ("KTc", 2), ("CKVT", 2), ("CQT", 6), ("KRT", 1), ("KRS", 1),
                  ("QTd", 8), ("QRd", 4), ("KTd", 8), ("KRd", 1)):
        FM[nm] = scr(nm, [g, 128, S])
    Va = scr("Va", [S, 256])
    Vb = scr("Vb", [S, 1024])
    Vc = scr("Vc", [S, 128])
    Vd = scr("Vd", [S, 1024])
    SZ = scr("SZ", [S, 4, 1024])
    IW = scr("IW", [S, 16], F32)
    BFt = scr("BFt", [S, 8], F32)
    COS = scr("COS", [128, S], F32)
    SIN = scr("SIN", [128, S], F32)
    BT = scr("BT", [128, 32, S])
    MT = scr("MT", [128, KC, S])

    uid = [0]

    def sbt(stack, name, shape, dt):
        uid[0] += 1
        return stack.enter_context(nc.sbuf_tensor(f"{name}_{uid[0]}", list(shape), dt))

    banks = [gstack.enter_context(nc.psum_tensor(f"bank{i}", [128, 512], F32)) for i in range(8)]
    bankB = [Buf() for _ in range(8)]

    ident = sbt(gstack, "ident", [128, 128], BF16)
    ones_bf = sbt(gstack, "ones_bf", [128, 128], BF16)
    CBm = sbt(gstack, "CBm", [128, 128], BF16)
    causN = sbt(gstack, "causN", [128, 128], F32)
    Utri = sbt(gstack, "Utri", [128, 128], F32)
    ones_f = sbt(gstack, "ones_f", [128, 128], F32)
    half_f = sbt(gstack, "half_f", [128, 128], F32)
    epsq = sbt(gstack, "epsq", [128, 1], F32)
    epsln = sbt(gstack, "epsln", [128, 1], F32)
    Bc = Buf()
    P.op(pool, lambda e: e.memset(ident[:], 1.0), writes=[Bc])
    P.op(pool, lambda e: e.affine_select(out=ident[:], in_=ident[:], pattern=[[-1, 128]], compare_op=ALU.is_equal,
                                         fill=0.0, base=0, channel_multiplier=1), reads=[Bc], writes=[Bc])
    P.op(pool, lambda e: e.memset(ones_bf[:], 1.0), writes=[Bc])
    P.op(pool, lambda e: e.memset(ones_f[:], 1.0), writes=[Bc])
    P.op(pool, lambda e: e.memset(half_f[:], 0.0), writes=[Bc])
    P.op(pool, lambda e: e.memset(half_f[0:64, :], 1.0), reads=[Bc], writes=[Bc])
    P.op(pool, lambda e: e.memset(CBm[:], 0.0), writes=[Bc])
    P.op(pool, lambda e: e.affine_select(out=CBm[:], in_=CBm[:], pattern=[[-1, 128]], compare_op=ALU.is_ge,
                                         fill=-1.0e9, base=0, channel_multiplier=1), reads=[Bc], writes=[Bc])
    P.op(pool, lambda e: e.memset(causN[:], 0.0), writes=[Bc])
    P.op(pool, lambda e: e.affine_select(out=causN[:], in_=causN[:], pattern=[[-1, 128]], compare_op=ALU.is_ge,
                                         fill=NEG, base=0, channel_multiplier=1), reads=[Bc], writes=[Bc])
    P.op(pool, lambda e: e.memset(Utri[:], 1.0), writes=[Bc])
    P.op(pool, lambda e: e.affine_select(out=Utri[:], in_=Utri[:], pattern=[[1, 128]], compare_op=ALU.is_ge,
                                         fill=0.0, base=0, channel_multiplier=-1), reads=[Bc], writes=[Bc])
    P.op(pool, lambda e: e.memset(epsq[:], RMS_EPS), writes=[Bc])
    P.op(pool, lambda e: e.memset(epsln[:], LN_EPS), writes=[Bc])
    P.barrier()

    rr = {"evac": 0, "bank": 0, "tb": (6, 7)}

    def evac_eng():
        rr["evac"] += 1
        return act if rr["evac"] % 2 else dve

    def copy_op(eng, out, in_):
        if eng is act:
            return lambda e: e.copy(out=out, in_=in_)
        return lambda e: e.tensor_copy(out=out, in_=in_)

    def piece_list():
        pcs = []
        sg = 0
        for nm, n in (("a_q", 1024),):
            pcs += [(0, 0, SEG[nm], 512), (1, 0, SEG[nm] + 512, 512)]
        pcs += [(2, 0, SEG["a_k"], 256), (2, 256, SEG["a_ik"], 64), (2, 320, SEG["a_ik"], 64)]
        pcs += [(3, 0, SEG["a_iq"], 512), (4, 0, SEG["a_iq"] + 512, 512)]
        pcs += [(5, 0, SEG["b_q"], 512), (6, 0, SEG["b_q"] + 512, 512)]
        pcs += [(7, 0, SEG["b_k"], 512), (8, 0, SEG["b_k"] + 512, 512)]
        pcs += [(9, 0, SEG["c_q"], 512), (10, 0, SEG["c_q"] + 512, 512)]
        ck = SEG["c_k"]
        pcs += [(11, 0, ck, 64), (11, 64, ck, 64), (11, 128, ck + 64, 64), (11, 192, ck + 64, 64),
                (11, 256, SEG["d_ckv"], 256)]
        pcs += [(12, 0, SEG["d_cq"], 512)]
        kr = SEG["d_kr"]
        pcs += [(13, 0, SEG["d_cq"] + 512, 256), (13, 256, kr, 64), (13, 320, kr, 64),
                (13, 384, kr + 32, 32), (13, 416, kr, 32), (13, 448, kr + 32, 32), (13, 480, kr, 32)]
        pcs += [(14, 0, SEG["a_v"], 256), (14, 256, SEG["a_iw"], 16), (14, 272, SEG["b_f"], 8)]
        pcs += [(15, 0, SEG["a_z"], 512), (16, 0, SEG["a_z"] + 512, 512)]
        pcs += [(17, 0, SEG["b_v"], 512), (18, 0, SEG["b_v"] + 512, 512)]
        pcs += [(19, 0, SEG["b_z"], 512), (20, 0, SEG["b_z"] + 512, 512)]
        pcs += [(21, 0, SEG["c_v"], 128)]
        pcs += [(22, 0, SEG["c_z"], 512), (23, 0, SEG["c_z"] + 512, 512)]
        pcs += [(24, 0, SEG["d_z"], 512), (25, 0, SEG["d_z"] + 512, 512)]
        return pcs

    def stage_W(l):
        wv = w_in[l].rearrange("(kc p) c -> p kc c", p=128)
        for (sg, dc, sc_, n) in piece_list():
            step = 8
            for k0 in range(0, KC, step):
                P.dma(pool, WIN[sg][:, k0:k0 + step, dc:dc + n], wv[:, k0:k0 + step, sc_:sc_ + n])
        for n in range(4):
            gv = w_gate[l, n].rearrange("(kc p) c -> p kc c", p=128)
            bv = w_branch[l, n].rearrange("(kc p) c -> p kc c", p=128)
            for cgg in range(8):
                for k0 in range(0, KC, 8):
                    P.dma(pool, WG[n * 8 + cgg][:, k0:k0 + 8, :], gv[:, k0:k0 + 8, cgg * 512:(cgg + 1) * 512])
                P.dma(pool, WB[n * 8 + cgg][:, :, :], bv[:, :, cgg * 512:(cgg + 1) * 512])
        ov = w_out[l].rearrange("(kc p) c -> p kc c", p=128)
        for cgg in range(8):
            for k0 in range(0, KC, 8):
                P.dma(pool, WO[cgg][:, k0:k0 + 8, :], ov[:, k0:k0 + 8, cgg * 512:(cgg + 1) * 512])

    def stage_R():
        with ExitStack() as st:
            pos_i = sbt(st, "pos_i", [128, S], I32)
            ang = sbt(st, "ang", [128, S], F32)
            tmp = sbt(st, "rtmp", [128, S], F32)
            invf = sbt(st, "invf", [128, 1], F32)
            sgn = sbt(st, "sgn", [128, 1], F32)
            negpi = sbt(st, "negpi", [128, 1], F32)
            B = Buf()
            cst = sbt(st, "cst", [128, 4], F32)
            P.dma(sp, pos_i[:], pos_in[0].partition_broadcast(128), writes=[B])
            P.dma(sp, cst[:], consts, writes=[B], join=True)
            P.op(dve, lambda e: e.tensor_copy(out=invf[:], in_=cst[:, 0:1]), reads=[B], writes=[B])
            P.op(dve, lambda e: e.tensor_copy(out=sgn[:], in_=cst[:, 1:2]), reads=[B], writes=[B])
            P.op(pool, lambda e: e.memset(negpi[:], -math.pi), writes=[B])
            P.op(dve, lambda e: e.tensor_copy(out=ang[:], in_=pos_i[:]), reads=[B], writes=[B])
            P.op(dve, lambda e: e.tensor_scalar(out=ang[:], in0=ang[:], scalar1=invf[:, 0:1], scalar2=None,
                                                op0=ALU.mult), reads=[B], writes=[B])
            two_pi = 2.0 * math.pi
            ki = sbt(st, "rki", [128, S], I32)
            kf = sbt(st, "rkf", [128, S], F32)

            def reduce_sin(src, dst):
                P.op(dve, lambda e: e.tensor_scalar(out=kf[:], in0=src[:], scalar1=1.0 / two_pi, scalar2=None, op0=ALU.mult),
                     reads=[B], writes=[B])
                P.op(dve, lambda e: e.tensor_copy(out=ki[:], in_=kf[:]), reads=[B], writes=[B])
                P.op(dve, lambda e: e.tensor_copy(out=kf[:], in_=ki[:]), reads=[B], writes=[B])
                P.op(dve, lambda e: e.scalar_tensor_tensor(out=dst[:], in0=kf[:], scalar=-two_pi, in1=src[:], op0=ALU.mult,
                                                           op1=ALU.add), reads=[B], writes=[B])
                P.op(dve, lambda e: e.tensor_scalar(out=kf[:], in0=dst[:], scalar1=math.pi, scalar2=two_pi, op0=ALU.is_gt,
                                                    op1=ALU.mult), reads=[B], writes=[B])
                P.op(dve, lambda e: e.tensor_tensor(out=dst[:], in0=dst[:], in1=kf[:], op=ALU.subtract), reads=[B], writes=[B])
                P.op(dve, lambda e: e.tensor_scalar(out=kf[:], in0=dst[:], scalar1=-math.pi, scalar2=two_pi, op0=ALU.is_lt,
                                                    op1=ALU.mult), reads=[B], writes=[B])
                P.op(dve, lambda e: e.tensor_tensor(out=dst[:], in0=dst[:], in1=kf[:], op=ALU.add), reads=[B], writes=[B])
                P.op(act, lambda e: e.activation(out=dst[:], in_=dst[:], func=AF.Sin), reads=[B], writes=[B])

            reduce_sin(ang, tmp)
            P.op(dve, lambda e: e.tensor_scalar(out=tmp[:], in0=tmp[:], scalar1=sgn[:, 0:1], scalar2=None,
                                                op0=ALU.mult), reads=[B], writes=[B])
            P.dma(sp, SIN, tmp[:], reads=[B], writes=[B])
            P.op(dve, lambda e: e.tensor_scalar(out=ang[:], in0=ang[:], scalar1=0.5 * math.pi, scalar2=None, op0=ALU.add),
                 reads=[B], writes=[B])
            reduce_sin(ang, ang)
            P.dma(sp, COS, ang[:], reads=[B], writes=[B])
            P.barrier()

    def transpose_block(src_bf, nchunks, dst_sb, dstB, srcBs):
        for q0 in range(0, nchunks, 8):
            nq = min(8, nchunks - q0)
            bi = rr["tb"][rr["bank"] % len(rr["tb"])]
            rr["bank"] += 1
            bk = banks[bi][:].bitcast(BF16)
            for j in range(nq):
                c = q0 + j
                P.op(pe, lambda e, c=c, j=j: e.transpose(out=bk[:, j * 128:(j + 1) * 128],
                                                         in_=src_bf[:, c * 128:(c + 1) * 128], identity=ident[:]),
                     reads=list(srcBs), writes=[bankB[bi]], inc=(j == nq - 1))
            eng = evac_eng()
            dB = dstB[q0 // 8] if isinstance(dstB, list) else dstB
            P.op(eng, copy_op(eng, dst_sb[:, q0:q0 + nq, :].rearrange("p c t -> p (c t)"), bk[:, 0:nq * 128]),
                 reads=[bankB[bi]], writes=[dB])

    def stage_0(src):
        with ExitStack() as st:
            xf = [sbt(st, f"s0xf{i}", [128, D], F32) for i in range(2)]
            xb = [sbt(st, f"s0xb{i}", [128, D], BF16) for i in range(2)]
            xt = [sbt(st, f"s0xt{i}", [128, KC, 128], BF16) for i in range(2)]
            Bxf = [Buf() for _ in range(2)]
            Bxb = [(Buf(), Buf()) for _ in range(2)]
            Bxt = [[Buf() for _ in range(4)] for _ in range(2)]
            for blk in range(NB):
                i = blk % 2
                P.dma(sp, xf[i][:], src[blk * 128:(blk + 1) * 128, :], writes=[Bxf[i]])
                P.op(act, lambda e, i=i: e.copy(out=xb[i][:, 0:2048], in_=xf[i][:, 0:2048]), reads=[Bxf[i]],
                     writes=[Bxb[i][0]])
                P.op(dve, lambda e, i=i: e.tensor_copy(out=xb[i][:, 2048:D], in_=xf[i][:, 2048:D]), reads=[Bxf[i]],
                     writes=[Bxb[i][1]])
                transpose_block(xb[i], KC, xt[i], Bxt[i], Bxb[i])
                for k0 in range(0, KC, 8):
                    P.dma(sp, XT[:, k0:k0 + 8, blk * 128:(blk + 1) * 128], xt[i][:, k0:k0 + 8, :], reads=[Bxt[i][k0 // 8]])
        P.barrier()

    FM_SG = {
        0: [(0, 128, "QTa", 0), (128, 128, "QTa", 1), (256, 128, "QTa", 2), (384, 128, "QTa", 3)],
        1: [(0, 128, "QTa", 4), (128, 128, "QTa", 5), (256, 128, "QTa", 6), (384, 128, "QTa", 7)],
        2: [(0, 128, "KTa", 0), (128, 128, "KTa", 1), (256, 128, "IKT", 0)],
        3: [(i * 128, 128, "IQT", i) for i in range(4)],
        4: [(i * 128, 128, "IQT", 4 + i) for i in range(4)],
        5: [(i * 128, 128, "QTb", i) for i in range(4)],
        6: [(i * 128, 128, "QTb", 4 + i) for i in range(4)],
        7: [(i * 128, 128, "KTb", i) for i in range(4)],
        8: [(i * 128, 128, "KTb", 4 + i) for i in range(4)],
        9: [(i * 128, 128, "QTc", i) for i in range(4)],
        10: [(i * 128, 128, "QTc", 4 + i) for i in range(4)],
        11: [(0, 128, "KTc", 0), (128, 128, "KTc", 1), (256, 128, "CKVT", 0), (384, 128, "CKVT", 1)],
        12: [(i * 128, 128, "CQT", i) for i in range(4)],
        13: [(0, 128, "CQT", 4), (128, 128, "CQT", 5), (256, 128, "KRT", 0), (384, 128, "KRS", 0)],
    }
    TM_SG = {
        14: ("mix", 280), 15: ("sz", 0, 0), 16: ("sz", 0, 512), 17: ("v", Vb, 0), 18: ("v", Vb, 512),
        19: ("sz", 1, 0), 20: ("sz", 1, 512), 21: ("v", Vc, 0, 128), 22: ("sz", 2, 0), 23: ("sz", 2, 512),
        24: ("sz", 3, 0), 25: ("sz", 3, 512),
    }

    def stage_1():
        TC = min(1024, S)
        with ExitStack() as st:
            xc = sbt(st, "s1xc", [128, KC, TC], BF16)
            wt = [sbt(st, f"s1wt{i}", [128, KC, 512], BF16) for i in range(2)]
            so = [sbt(st, f"s1so{i}", [128, 512], BF16) for i in range(4)]
            sf = [sbt(st, f"s1sf{i}", [128, 32], F32) for i in range(2)]
            Bxc = Buf()
            Bwt = [Buf(), Buf()]
            Bso = [Buf() for _ in range(4)]
            Bsf = [Buf() for _ in range(2)]
            wi = 0
            oi = 0
            for c in range(S // TC):
                t0c = c * TC
                for k0 in range(0, KC, 8):
                    P.dma(sp, xc[:, k0:k0 + 8, :], XT[:, k0:k0 + 8, t0c:t0c + TC], writes=[Bxc], join=(k0 > 0))
                import os
                for sg in [int(v) for v in os.environ.get('SGS', ','.join(str(i) for i in range(NSG))).split(',')]:
                    w = wt[wi % 2]
                    Bw = Bwt[wi % 2]
                    wi += 1
                    P.dma(sp, w[:], WIN[sg], writes=[Bw])
                    if sg in FM_SG:
                        for (c0, ncol, nm, gi) in FM_SG[sg]:
                            for th in range(TC // 512):
                                bi = 2 + rr["bank"] % 6
                                rr["bank"] += 1
                                for kc in range(KC):
                                    P.op(pe, lambda e, kc=kc, bi=bi, c0=c0, ncol=ncol, th=th, w=w: e.matmul(
                                        banks[bi][0:ncol, :], lhsT=w[:, kc, c0:c0 + ncol],
                                        rhs=xc[:, kc, th * 512:(th + 1) * 512], start=(kc == 0), stop=(kc == KC - 1)),
                                        reads=[Bw, Bxc], writes=[bankB[bi]], inc=(kc == KC - 1))
                                o = so[oi % 4]
                                Bo = Bso[oi % 4]
                                oi += 1
                                eng = evac_eng()
                                P.op(eng, copy_op(eng, o[0:ncol, :], banks[bi][0:ncol, :]), reads=[bankB[bi]], writes=[Bo])
                                P.dma(pool, FM[nm][gi][0:ncol, t0c + th * 512:t0c + (th + 1) * 512], o[0:ncol, :], reads=[Bo])
                    else:
                        spec = TM_SG[sg]
                        ncol = 280 if spec[0] == "mix" else (spec[3] if len(spec) > 3 else 512)
                        for tb in range(TC // 128):
                            bi = 2 + rr["bank"] % 6
                            rr["bank"] += 1
                            for kc in range(KC):
                                P.op(pe, lambda e, kc=kc, bi=bi, ncol=ncol, tb=tb, w=w: e.matmul(
                                    banks[bi][:, 0:ncol], lhsT=xc[:, kc, tb * 128:(tb + 1) * 128],
                                    rhs=w[:, kc, 0:ncol], start=(kc == 0), stop=(kc == KC - 1)),
                                    reads=[Bw, Bxc], writes=[bankB[bi]], inc=(kc == KC - 1))
                            r0 = t0c + tb * 128
                            o = so[oi % 4]
                            Bo = Bso[oi % 4]
                            oi += 1
                            if spec[0] == "sz":
                                P.op(act, lambda e, o=o, bi=bi: e.activation(out=o[:, :], in_=banks[bi][:, :], func=AF.Silu),
                                     reads=[bankB[bi]], writes=[Bo])
                                P.dma(pool, SZ[r0:r0 + 128, spec[1], spec[2]:spec[2] + 512], o[:, :], reads=[Bo])
                            elif spec[0] == "v":
                                eng = evac_eng()
                                P.op(eng, copy_op(eng, o[:, 0:ncol], banks[bi][:, 0:ncol]), reads=[bankB[bi]], writes=[Bo])
                                P.dma(pool, spec[1][r0:r0 + 128, spec[2]:spec[2] + ncol], o[:, 0:ncol], reads=[Bo])
                            else:
                                f = sf[tb % 2]
                                Bf = Bsf[tb % 2]
                                P.op(dve, lambda e, o=o, bi=bi: e.tensor_copy(out=o[:, 0:256], in_=banks[bi][:, 0:256]),
                                     reads=[bankB[bi]], writes=[Bo])
                                P.dma(pool, Va[r0:r0 + 128, :], o[:, 0:256], reads=[Bo])
                                P.op(dve, lambda e, f=f, bi=bi: e.tensor_scalar(out=f[:, 0:16], in0=banks[bi][:, 256:272],
                                                                                scalar1=1.0 / 32.0, scalar2=None, op0=ALU.mult),
                                     reads=[bankB[bi]], writes=[Bf])
                                P.op(dve, lambda e, f=f, bi=bi: e.tensor_copy(out=f[:, 16:24], in_=banks[bi][:, 272:280]),
                                     reads=[bankB[bi], Bf], writes=[Bf])
                                P.dma(pool, IW[r0:r0 + 128, :], f[:, 0:16], reads=[Bf])
                                P.dma(pool, BFt[r0:r0 + 128, :], f[:, 16:24], reads=[Bf])
        P.barrier()

    def stage_1b(l):
        with ExitStack() as st:
            wq_f = sbt(st, "bwqf", [128, 1536], F32)
            WQ = sbt(st, "bWQ", [128, 6, 1536], BF16)
            WQr = sbt(st, "bWQr", [128, 6, 512], BF16)
            WQs = sbt(st, "bWQs", [128, 6, 512], BF16)
            wk_f = sbt(st, "bwkf", [128, 2048], F32)
            WKV = sbt(st, "bWKV", [128, 2, 2048], BF16)
            WKn = sbt(st, "bWKn", [128, 2, 1024], BF16)
            WVv = sbt(st, "bWVv", [128, 2, 1024], BF16)
            gq = sbt(st, "bgq", [128, 6], F32)
            gk = sbt(st, "bgk", [128, 2], F32)
            Bw = Buf()
            Bg = Buf()
            Bf = Buf()
            P.dma(sp, gq[:], dq_gain[l].rearrange("(kc p) -> p kc", p=128), writes=[Bg], allow_slow_non_contiguous=True)
            P.dma(sp, gk[:], dkv_gain[l].rearrange("(kc p) -> p kc", p=128), writes=[Bg], join=True, allow_slow_non_contiguous=True)
            for kc in range(6):
                P.dma(sp, wq_f[:], dq_up[l, kc * 128:(kc + 1) * 128, :], writes=[Bf])
                P.op(dve, lambda e, kc=kc: e.tensor_scalar(out=WQ[:, kc, :], in0=wq_f[:], scalar1=gq[:, kc:kc + 1],
                                                           scalar2=None, op0=ALU.mult), reads=[Bf, Bg], writes=[Bw])
                v = WQ[:, kc, :].rearrange("p (h c) -> p h c", c=192)
                P.op(dve, lambda e, kc=kc, v=v: e.tensor_copy(out=WQr[:, kc, :].rearrange("p (h c) -> p h c", c=64),
                                                              in_=v[:, :, 128:192]), reads=[Bw], writes=[Bw])
                P.op(dve, lambda e, kc=kc, v=v: e.tensor_copy(
                    out=WQs[:, kc, :].rearrange("p (h c) -> p h c", c=64)[:, :, 0:32], in_=v[:, :, 160:192]),
                    reads=[Bw], writes=[Bw])
                P.op(dve, lambda e, kc=kc, v=v: e.tensor_copy(
                    out=WQs[:, kc, :].rearrange("p (h c) -> p h c", c=64)[:, :, 32:64], in_=v[:, :, 128:160]),
                    reads=[Bw], writes=[Bw])
            for kc in range(2):
                P.dma(sp, wk_f[:], dkv_up[l, kc * 128:(kc + 1) * 128, :], writes=[Bf])
                P.op(dve, lambda e, kc=kc: e.tensor_scalar(out=WKV[:, kc, :], in0=wk_f[:], scalar1=gk[:, kc:kc + 1],
                                                           scalar2=None, op0=ALU.mult), reads=[Bf, Bg], writes=[Bw])
                v = WKV[:, kc, :].rearrange("p (h c) -> p h c", c=256)
                P.op(dve, lambda e, kc=kc, v=v: e.tensor_copy(out=WKn[:, kc, :].rearrange("p (h c) -> p h c", c=128),
                                                              in_=v[:, :, 0:128]), reads=[Bw], writes=[Bw])
                P.op(dve, lambda e, kc=kc, v=v: e.tensor_copy(out=WVv[:, kc, :].rearrange("p (h c) -> p h c", c=128),
                                                              in_=v[:, :, 128:256]), reads=[Bw], writes=[Bw])
            cq = [sbt(st, f"bcq{i}", [128, 6, 512], BF16) for i in range(2)]
            ckv = [sbt(st, f"bckv{i}", [128, 2, 512], BF16) for i in range(2)]
            kr = [sbt(st, f"bkr{i}", [128, 512], BF16) for i in range(2)]
            krs = [sbt(st, f"bkrs{i}", [128, 512], BF16) for i in range(2)]
            cs = [sbt(st, f"bcos{i}", [128, 512], F32) for i in range(2)]
            sn = [sbt(st, f"bsin{i}", [128, 512], F32) for i in range(2)]
            Bin = [Buf() for _ in range(2)]
            sq = sbt(st, "bsq", [128, 8, 512], BF16)
            Bsq = Buf()
            rbq = sbt(st, "brbq", [128, 512], F32)
            rbk = sbt(st, "brbk", [128, 512], F32)
            rtm = sbt(st, "brtm", [128, 4], F32)
            Brb = Buf()
            ta = sbt(st, "bta", [128, 512], F32)
            tb_ = sbt(st, "btb", [128, 512], F32)
            Bt = Buf()
            so = [sbt(st, f"bso{i}", [128, 512], BF16) for i in range(4)]
            Bso = [Buf() for _ in range(4)]
            oi = 0

            def nb():
                bi = 2 + rr["bank"] % 6
                rr["bank"] += 1
                return bi

            for ch in range(NCH):
                i = ch % 2
                t0 = ch * 512
                B = Bin[i]
                P.dma(sp, cq[i][:], FM["CQT"][:, :, t0:t0 + 512].rearrange("g p t -> p g t"), writes=[B])
                P.dma(sp, ckv[i][:], FM["CKVT"][:, :, t0:t0 + 512].rearrange("g p t -> p g t"), writes=[B], join=True)
                P.dma(sp, kr[i][:], FM["KRT"][0][:, t0:t0 + 512], writes=[B], join=True)
                P.dma(sp, krs[i][:], FM["KRS"][0][:, t0:t0 + 512], writes=[B], join=True)
                P.dma(sp, cs[i][:], COS[:, t0:t0 + 512], writes=[B], join=True)
                P.dma(sp, sn[i][:], SIN[:, t0:t0 + 512], writes=[B], join=True)
                P.op(act, lambda e, i=i: e.activation(out=sq[:, 0:6, :], in_=cq[i][:], func=AF.Square), reads=[B], writes=[Bsq])
                P.op(act, lambda e, i=i: e.activation(out=sq[:, 6:8, :], in_=ckv[i][:], func=AF.Square), reads=[B, Bsq], writes=[Bsq])
                bq = nb()
                for kc in range(6):
                    P.op(pe, lambda e, kc=kc, bq=bq: e.matmul(banks[bq][:, :], lhsT=ones_bf[:], rhs=sq[:, kc, :],
                                                              start=(kc == 0), stop=(kc == 5)),
                         reads=[Bsq], writes=[bankB[bq]], inc=(kc == 5))
                P.op(act, lambda e, bq=bq: e.activation(out=rbq[:], in_=banks[bq][:, :], func=AF.Sqrt, bias=epsq[:, 0:1],
                                                        scale=1.0 / 768.0), reads=[bankB[bq]], writes=[Brb])
                P.op(dve, lambda e: e.reciprocal(out=rbq[:], in_=rbq[:]), reads=[Brb], writes=[Brb])
                bk = nb()
                for kc in range(2):
                    P.op(pe, lambda e, kc=kc, bk=bk: e.matmul(banks[bk][:, :], lhsT=ones_bf[:], rhs=sq[:, 6 + kc, :],
                                                              start=(kc == 0), stop=(kc == 1)),
                         reads=[Bsq], writes=[bankB[bk]], inc=(kc == 1))
                P.op(act, lambda e, bk=bk: e.activation(out=rbk[:], in_=banks[bk][:, :], func=AF.Sqrt, bias=epsq[:, 0:1],
                                                        scale=1.0 / 256.0), reads=[bankB[bk], Brb], writes=[Brb])
                P.op(dve, lambda e: e.reciprocal(out=rbk[:], in_=rbk[:]), reads=[Brb], writes=[Brb])
                bt = nb()
                for tb in range(4):
                    for kc in range(2):
                        P.op(pe, lambda e, kc=kc, tb=tb, bt=bt: e.matmul(
                            banks[bt][:, tb:tb + 1], lhsT=sq[:, 6 + kc, tb * 128:(tb + 1) * 128], rhs=ones_bf[:, 0:1],
                            start=(kc == 0 and tb == 0), stop=(kc == 1), skip_group_check=True),
                            reads=[Bsq], writes=[bankB[bt]], inc=(kc == 1 and tb == 3))
                P.op(act, lambda e, bt=bt: e.activation(out=rtm[:], in_=banks[bt][:, 0:4], func=AF.Sqrt, bias=epsq[:, 0:1],
                                                        scale=1.0 / 256.0), reads=[bankB[bt], Brb], writes=[Brb])
                P.op(dve, lambda e: e.reciprocal(out=rtm[:], in_=rtm[:]), reads=[Brb], writes=[Brb])

                def out_fm(bi, rb, dst):
                    nonlocal oi
                    o = so[oi % 4]
                    Bo = Bso[oi % 4]
                    oi += 1
                    P.op(dve, lambda e: e.tensor_tensor(out=o[:, :], in0=banks[bi][:, :], in1=rb[:], op=ALU.mult),
                         reads=[bankB[bi], Brb], writes=[Bo])
                    P.dma(pool, dst, o[:, :], reads=[Bo])

                for h in range(8):
                    bi = nb()
                    for kc in range(6):
                        P.op(pe, lambda e, kc=kc, bi=bi, h=h, i=i: e.matmul(
                            banks[bi][:, :], lhsT=WQ[:, kc, h * 192:h * 192 + 128], rhs=cq[i][:, kc, :],
                            start=(kc == 0), stop=(kc == 5)), reads=[Bw, B], writes=[bankB[bi]], inc=(kc == 5))
                    out_fm(bi, rbq, FM["QTd"][h][:, t0:t0 + 512])
                for m in range(4):
                    b1 = nb()
                    b2 = nb()
                    for (bb, W_) in ((b1, WQr), (b2, WQs)):
                        for kc in range(6):
                            P.op(pe, lambda e, kc=kc, bb=bb, W_=W_, m=m, i=i: e.matmul(
                                banks[bb][:, :], lhsT=W_[:, kc, m * 128:(m + 1) * 128], rhs=cq[i][:, kc, :],
                                start=(kc == 0), stop=(kc == 5)), reads=[Bw, B], writes=[bankB[bb]], inc=(kc == 5))
                    P.op(dve, lambda e, b1=b1, i=i: e.tensor_tensor(out=ta[:], in0=banks[b1][:, :], in1=cs[i][:], op=ALU.mult),
                         reads=[bankB[b1], B], writes=[Bt])
                    P.op(dve, lambda e, b2=b2, i=i: e.tensor_tensor(out=tb_[:], in0=banks[b2][:, :], in1=sn[i][:], op=ALU.mult),
                         reads=[bankB[b2], B, Bt], writes=[Bt])
                    P.op(dve, lambda e: e.tensor_tensor(out=ta[:], in0=ta[:], in1=tb_[:], op=ALU.add), reads=[Bt], writes=[Bt])
                    o = so[oi % 4]
                    Bo = Bso[oi % 4]
                    oi += 1
                    P.op(dve, lambda e, o=o: e.tensor_tensor(out=o[:, :], in0=ta[:], in1=rbq[:], op=ALU.mult),
                         reads=[Bt, Brb], writes=[Bo])
                    P.dma(pool, FM["QRd"][m][:, t0:t0 + 512], o[:, :], reads=[Bo])
                for h in range(8):
                    bi = nb()
                    for kc in range(2):
                        P.op(pe, lambda e, kc=kc, bi=bi, h=h, i=i: e.matmul(
                            banks[bi][:, :], lhsT=WKn[:, kc, h * 128:(h + 1) * 128], rhs=ckv[i][:, kc, :],
                            start=(kc == 0), stop=(kc == 1)), reads=[Bw, B], writes=[bankB[bi]], inc=(kc == 1))
                    out_fm(bi, rbk, FM["KTd"][h][:, t0:t0 + 512])
                for tb in range(4):
                    for hf in range(2):
                        bi = nb()
                        for kc in range(2):
                            P.op(pe, lambda e, kc=kc, bi=bi, hf=hf, tb=tb, i=i: e.matmul(
                                banks[bi][:, :], lhsT=ckv[i][:, kc, tb * 128:(tb + 1) * 128],
                                rhs=WVv[:, kc, hf * 512:(hf + 1) * 512], start=(kc == 0), stop=(kc == 1)),
                                reads=[Bw, B], writes=[bankB[bi]], inc=(kc == 1))
                        o = so[oi % 4]
                        Bo = Bso[oi % 4]
                        oi += 1
                        P.op(dve, lambda e, o=o, bi=bi, tb=tb: e.tensor_scalar(out=o[:, :], in0=banks[bi][:, :],
                                                                               scalar1=rtm[:, tb:tb + 1], scalar2=None, op0=ALU.mult),
                             reads=[bankB[bi], Brb], writes=[Bo])
                        P.dma(pool, Vd[t0 + tb * 128:t0 + (tb + 1) * 128, hf * 512:(hf + 1) * 512], o[:, :], reads=[Bo])
                P.op(dve, lambda e, i=i: e.tensor_tensor(out=ta[:], in0=kr[i][:], in1=cs[i][:], op=ALU.mult),
                     reads=[B, Bt], writes=[Bt])
                P.op(dve, lambda e, i=i: e.tensor_tensor(out=tb_[:], in0=krs[i][:], in1=sn[i][:], op=ALU.mult),
                     reads=[B, Bt], writes=[Bt])
                o = so[oi % 4]
                Bo = Bso[oi % 4]
                oi += 1
                P.op(dve, lambda e, o=o: e.tensor_tensor(out=o[:, :], in0=ta[:], in1=tb_[:], op=ALU.add),
                     reads=[Bt], writes=[Bo])
                P.dma(pool, FM["KRd"][0][:, t0:t0 + 512], o[:, :], reads=[Bo])
        P.barrier()

    ST_BANKS = (0, 1)
    O_BANKS = (2, 3)

    class AttnCtx:
        pass

    def attn_stream(st, tiles, sbanks=(0, 1, 4, 5)):
        NS = len(sbanks)
        LA = NS - 1
        NPT = 2 * NS
        pT = [sbt(st, f"apT{i}", [128, 128], BF16) for i in range(NPT)]
        BpT = [Buf() for _ in range(NPT)]
        pre = [sbt(st, f"apre{i}", [128, 128], F32) for i in range(NS)]
        Bpre = [Buf() for _ in range(NS)]
        k = 0
        pend = []

        def emit_front(t):
            nonlocal k
            slot = k % NS
            pslot = k % NPT
            k += 1
            bidx = sbanks[slot]
            stv = banks[bidx][:, 0:128]
            n = len(t["S"])
            for a, (lh, rh, rd) in enumerate(t["S"]):
                P.op(pe, lambda e, lh=lh, rh=rh, a=a: e.matmul(stv, lhsT=lh, rhs=rh, start=(a == 0), stop=(a == n - 1)),
                     reads=rd, writes=[bankB[bidx]], inc=(a == n - 1))
            src = stv
            srcB = bankB[bidx]
            scale = t["scale"]
            if t.get("pre") is not None:
                P.op(dve, lambda e: e.scalar_tensor_tensor(out=pre[slot][:], in0=stv, scalar=scale, in1=t["pre"],
                                                           op0=ALU.mult, op1=ALU.add),
                     reads=[srcB] + t.get("pre_reads", []), writes=[Bpre[slot]])
                src = pre[slot][:]
                srcB = Bpre[slot]
                scale = 1.0
            bias = t.get("bias")
            if bias is None:
                P.op(act, lambda e: e.activation(out=pT[pslot][:], in_=src, func=AF.Exp, scale=scale),
                     reads=[srcB], writes=[BpT[pslot]])
            else:
                P.op(act, lambda e: e.activation(out=pT[pslot][:], in_=src, func=AF.Exp, bias=bias, scale=scale),
                     reads=[srcB] + t.get("bias_reads", []), writes=[BpT[pslot]])
            return pslot

        def emit_back(t, pslot):
            ob = t["obank"]
            P.op(pe, lambda e: e.matmul(banks[ob][:, 0:t["ow"]], lhsT=pT[pslot][:], rhs=t["pv_rhs"], start=t["start"],
                                        stop=t["stop"]),
                 reads=[BpT[pslot]] + t["pv_reads"], writes=[bankB[ob]], inc=t["stop"])

        for t in tiles:
            if t["kind"] == "call":
                while pend:
                    tt, sl = pend.pop(0)
                    emit_back(tt, sl)
                t["fn"]()
                continue
            slot = emit_front(t)
            pend.append((t, slot))
            if len(pend) > LA:
                tt, sl = pend.pop(0)
                emit_back(tt, sl)
        while pend:
            tt, sl = pend.pop(0)
            emit_back(tt, sl)

    def store_branch(st_pool, n, blk, br, Bbr, brT, BbrT):
        transpose_block(br, 8, brT, BbrT, [Bbr])
        P.dma(pool, BT[:, n * 8:(n + 1) * 8, blk * 128:(blk + 1) * 128], brT[:], reads=[BbrT])

    def finalize(ob, d, den_extra, sz_ap, szB, out_ap, outB, rinv, Brinv):
        if den_extra is None:
            P.op(dve, lambda e: e.reciprocal(out=rinv[:, 0:1], in_=banks[ob][:, d:d + 1]), reads=[bankB[ob]], writes=[Brinv])
        else:
            P.op(dve, lambda e: e.tensor_tensor(out=rinv[:, 0:1], in0=banks[ob][:, d:d + 1], in1=den_extra[0], op=ALU.add),
                 reads=[bankB[ob]] + den_extra[1], writes=[Brinv])
            P.op(dve, lambda e: e.reciprocal(out=rinv[:, 0:1], in_=rinv[:, 0:1]), reads=[Brinv], writes=[Brinv])
        P.op(dve, lambda e: e.scalar_tensor_tensor(out=out_ap, in0=banks[ob][:, 0:d], scalar=rinv[:, 0:1], in1=sz_ap,
                                                   op0=ALU.mult, op1=ALU.mult),
             reads=[bankB[ob], Brinv, szB], writes=[outB])

    def dense_mixer(l, which):
        n_br = 1 if which == "B" else 3
        KT = FM["KTb"] if which == "B" else FM["KTd"]
        QT = FM["QTb"] if which == "B" else FM["QTd"]
        Vsrc = Vb if which == "B" else Vd
        scale = 128 ** -0.5 if which == "B" else 192 ** -0.5
        with ExitStack() as st:
            VD = 128
            KTs = sbt(st, "dKT", [128, 8, S], BF16)
            Vaug = sbt(st, "dV", [128, NB, 8, 129], BF16)
            Bres = Buf()
            for h in range(8):
                P.dma(sp, KTs[:, h, :], KT[h], writes=[Bres], join=(h > 0))
            Bv0 = Buf()
            for b0 in range(NB):
                P.op(pool, lambda e, b0=b0: e.memset(Vaug[:, b0, :, VD:VD + 1], 1.0), writes=[Bv0], inc=(b0 == NB - 1))
            for b0 in range(NB):
                P.dma(sp, Vaug[:, b0, :, 0:128],
                      Vsrc[b0 * 128:(b0 + 1) * 128, :].rearrange("p (h c) -> p h c", c=128),
                      reads=[Bv0], writes=[Bres], join=True)
            if which == "D":
                KRs = sbt(st, "dKR", [128, S], BF16)
                P.dma(sp, KRs[:], FM["KRd"][0], writes=[Bres], join=True)
            else:
                bf_sb = sbt(st, "fbf", [128, NB, 8], F32)
                fb = sbt(st, "ffb", [128, 8], F32)
                lf = sbt(st, "flf", [128, NB, 8], F32)
                cum = sbt(st, "fcum", [128, NB, 8], F32)
                tot = sbt(st, "ftot", [128, NB, 8], F32)
                toth = sbt(st, "ftoth", [128, NB, 8], F32)
                off = sbt(st, "foff", [128, NB + 1, 8], F32)
                cposH = sbt(st, "fcpos", [128, 8, NB], F32)
                Bf = Buf()
                P.dma(sp, bf_sb[:], BFt.rearrange("(b p) h -> p b h", p=128), writes=[Bf])
                P.dma(sp, fb[:], f_bias[l].partition_broadcast(128), writes=[Bf], join=True)
                for b in range(NB):
                    P.op(dve, lambda e, b=b: e.tensor_tensor(out=bf_sb[:, b, :], in0=bf_sb[:, b, :], in1=fb[:], op=ALU.add),
                         reads=[Bf], writes=[Bf], inc=(b == NB - 1))
                P.op(act, lambda e: e.activation(out=lf[:], in_=bf_sb[:], func=AF.Exp, scale=-1.0), reads=[Bf], writes=[Bf])
                P.op(act, lambda e: e.activation(out=lf[:], in_=lf[:], func=AF.Ln, bias=1.0, scale=1.0), reads=[Bf], writes=[Bf])
                lf2 = lf[:].rearrange("p b h -> p (b h)")
                for c0 in range(0, NB * 8, 512):
                    cn = min(512, NB * 8 - c0)
                    P.op(pe, lambda e, c0=c0, cn=cn: e.matmul(banks[4][:, 0:cn], lhsT=Utri[:], rhs=lf2[:, c0:c0 + cn],
                                                              start=True, stop=True), reads=[Bf], writes=[bankB[4]])
                    P.op(dve, lambda e, c0=c0, cn=cn: e.tensor_copy(out=cum[:].rearrange("p b h -> p (b h)")[:, c0:c0 + cn],
                                                                    in_=banks[4][:, 0:cn]), reads=[bankB[4]], writes=[Bf])
                    P.op(pe, lambda e, c0=c0, cn=cn: e.matmul(banks[5][:, 0:cn], lhsT=ones_f[:], rhs=lf2[:, c0:c0 + cn],
                                                              start=True, stop=True), reads=[Bf], writes=[bankB[5]])
                    P.op(dve, lambda e, c0=c0, cn=cn: e.tensor_copy(out=tot[:].rearrange("p b h -> p (b h)")[:, c0:c0 + cn],
                                                                    in_=banks[5][:, 0:cn]), reads=[bankB[5]], writes=[Bf])
                    P.op(pe, lambda e, c0=c0, cn=cn: e.matmul(banks[6][:, 0:cn], lhsT=half_f[:], rhs=lf2[:, c0:c0 + cn],
                                                              start=True, stop=True), reads=[Bf], writes=[bankB[6]])
                    P.op(dve, lambda e, c0=c0, cn=cn: e.tensor_copy(out=toth[:].rearrange("p b h -> p (b h)")[:, c0:c0 + cn],
                                                                    in_=banks[6][:, 0:cn]), reads=[bankB[6]], writes=[Bf])
                P.op(dve, lambda e: e.memset(off[:, 0, :], 0.0), reads=[Bf], writes=[Bf])
                for b in range(NB):
                    P.op(dve, lambda e, b=b: e.tensor_tensor(out=off[:, b + 1, :], in0=off[:, b, :], in1=tot[:, b, :], op=ALU.add),
                         reads=[Bf], writes=[Bf])
                P.op(dve, lambda e: e.tensor_tensor(out=toth[:], in0=toth[:], in1=off[:, 0:NB, :], op=ALU.add), reads=[Bf], writes=[Bf])
                P.op(dve, lambda e: e.tensor_tensor(out=cposH[:].rearrange("p h b -> p b h"), in0=cum[:], in1=off[:, 0:NB, :],
                                                    op=ALU.add), reads=[Bf], writes=[Bf])
                biasT = [sbt(st, f"fbias{i}", [128, NB], F32) for i in range(2)]
                BbiasT = [Buf() for _ in range(2)]
            if "VDBG" in dbg and which == "B":
                VDBG = nc.dram_tensor("VDBG", [128, NB * 8 * 129], BF16, kind="ExternalOutput").ap()
                P.dma(sp, VDBG, Vaug[:].rearrange("p b h c -> p (b h c)"), reads=[Bres, Bv0])
                KDBG = nc.dram_tensor("KDBG", [128, 8 * S], BF16, kind="ExternalOutput").ap()
                P.dma(sp, KDBG, KTs[:].rearrange("p h t -> p (h t)"), reads=[Bres, Bv0])
            qi = [sbt(st, f"dq{i}", [128, 8, 128], BF16) for i in range(2)]
            Bq = [Buf() for _ in range(2)]
            if which == "D":
                qr = [sbt(st, f"dqr{i}", [128, 4, 128], BF16) for i in range(2)]
            szs = [sbt(st, f"dsz{i}", [128, 1024], BF16) for i in range(2)]
            br = [sbt(st, f"dbr{i}", [128, 1024], BF16) for i in range(2)]
            Bbr = [Buf() for _ in range(2)]
            brT = [sbt(st, f"dbrT{i}", [128, 8, 128], BF16) for i in range(2)]
            BbrT = [Buf() for _ in range(2)]
            rinv = [sbt(st, f"drinv{i}", [128, 1], F32) for i in range(2)]
            Brinv = [Buf() for _ in range(2)]
            tiles = []
            gi = 0
            import os
            ADBG = int(os.environ.get("ADBG", "100000"))
            for i in ([int(v) for v in os.environ['ABLK'].split(',')] if 'ABLK' in os.environ else range(min(NB, ADBG))):
                ib = i % 2

                def load(i=i, ib=ib):
                    blk = slice(i * 128, (i + 1) * 128)
                    P.dma(sp, qi[ib][:], QT[:, :, blk].rearrange("g p t -> p g t"), writes=[Bq[ib]])
                    if which == "D":
                        P.dma(sp, qr[ib][:], FM["QRd"][:, :, blk].rearrange("g p t -> p g t"), writes=[Bq[ib]], join=True)
                    P.dma(sp, szs[ib][:], SZ[blk, n_br, :], writes=[Bq[ib]], join=True)

                tiles.append(dict(kind="call", fn=load))
                for h in range(8):
                    ob = O_BANKS[gi % 2]
                    gidx = gi % 2
                    gi += 1
                    if which == "B":
                        bt_ = biasT[gidx]
                        Bb_ = BbiasT[gidx]

                        def mkbias(i=i, h=h, bt_=bt_, Bb_=Bb_):
                            P.op(dve, lambda e: e.tensor_scalar(out=bt_[:, 0:i + 1], in0=cposH[:, h, 0:i + 1],
                                                                scalar1=toth[:, i, h:h + 1], scalar2=None,
                                                                op0=ALU.subtract), reads=[Bf], writes=[Bb_])

                        tiles.append(dict(kind="call", fn=mkbias))
                    for j in range(i + 1):
                        ks = slice(j * 128, (j + 1) * 128)
                        Sl = [(KTs[:, h, ks], qi[ib][:, h, :], [Bres, Bq[ib]])]
                        if which == "D":
                            hp = (h % 2) * 64
                            Sl.append((KRs[hp:hp + 64, ks], qr[ib][hp:hp + 64, h // 2, :], [Bres, Bq[ib]]))
                        if j == i:
                            Sl.append((CBm[:], ident[:], []))
                        t = dict(kind="tile", S=Sl, scale=scale, pv_rhs=Vaug[:, j, h, :], pv_reads=[Bres], obank=ob, ow=129,
                                 start=(j == 0), stop=(j == i))
                        if which == "B":
                            t["bias"] = bt_[:, j:j + 1]
                            t["bias_reads"] = [Bb_]
                        tiles.append(t)

                    def fin(ob=ob, h=h, ib=ib, gidx=gidx):
                        finalize(ob, 128, None, szs[ib][:, h * 128:(h + 1) * 128], Bq[ib], br[ib][:, h * 128:(h + 1) * 128],
                                 Bbr[ib], rinv[gidx], Brinv[gidx])

                    tiles.append(dict(kind="call", fn=fin))

                def fin_blk(i=i, ib=ib):
                    store_branch(st, n_br, i, br[ib], Bbr[ib], brT[ib], BbrT[ib])

                tiles.append(dict(kind="call", fn=fin_blk))
            attn_stream(st, tiles)
        P.barrier()

    def mixer_C(l):
        scale = 64 ** -0.5
        with ExitStack() as st:
            VD = 64
            KTs = sbt(st, "cKT", [128, 2, S], BF16)
            Vaug = sbt(st, "cV", [128, NB, 2, 65], BF16)
            Bres = Buf()
            for g in range(2):
                P.dma(sp, KTs[:, g, :], FM["KTc"][g], writes=[Bres], join=(g > 0))
            Bv0 = Buf()
            for b0 in range(NB):
                P.op(pool, lambda e, b0=b0: e.memset(Vaug[:, b0, :, VD:VD + 1], 1.0), writes=[Bv0], inc=(b0 == NB - 1))
            for b0 in range(NB):
                P.dma(sp, Vaug[:, b0, :, 0:64],
                      Vc[b0 * 128:(b0 + 1) * 128, :].rearrange("p (h c) -> p h c", c=64), reads=[Bv0], writes=[Bres], join=True)
            dcur = sbt(st, "cdcur", [128, 128], F32)
            dprev = sbt(st, "cdprev", [128, 128], F32)
            di = sbt(st, "cdi", [128, 128], I32)
            BTc = sbt(st, "cBTc", [128, 2, 16, 128], F32)
            es = sbt(st, "ces", [128, 16], F32)
            Bb = Buf()
            P.op(pool, lambda e: e.iota(di[:], pattern=[[1, 128]], base=0, channel_multiplier=-1), writes=[Bb])
            P.op(dve, lambda e: e.tensor_copy(out=dcur[:], in_=di[:]), reads=[Bb], writes=[Bb])
            P.op(dve, lambda e: e.tensor_scalar(out=dprev[:], in0=dcur[:], scalar1=128.0, scalar2=None, op0=ALU.add),
                 reads=[Bb], writes=[Bb])
            mcur = sbt(st, "cmcur", [128, 128], F32)
            mprev = sbt(st, "cmprev", [128, 128], F32)
            P.op(pool, lambda e: e.memset(mcur[:], 0.0), writes=[Bb], join=True)
            P.op(pool, lambda e: e.memset(mprev[:], 0.0), writes=[Bb], join=True)
            P.op(pool, lambda e: e.affine_select(out=mcur[:], in_=mcur[:], pattern=[[1, 128]], compare_op=ALU.is_ge,
                                                 fill=-30000.0, base=0, channel_multiplier=-1), reads=[Bb], writes=[Bb])
            P.op(pool, lambda e: e.affine_select(out=mprev[:], in_=mprev[:], pattern=[[-1, 128]], compare_op=ALU.is_ge,
                                                 fill=-30000.0, base=-1, channel_multiplier=1), reads=[Bb], writes=[Bb])
            for h in range(16):
                slope = 2.0 ** (-8.0 * (h + 1) / 16)
                P.op(dve, lambda e, h=h, slope=slope: e.scalar_tensor_tensor(out=BTc[:, 1, h, :], in0=dcur[:], scalar=-slope,
                                                                             in1=mcur[:], op0=ALU.mult, op1=ALU.add),
                     reads=[Bb], writes=[Bb])
                P.op(dve, lambda e, h=h, slope=slope: e.scalar_tensor_tensor(out=BTc[:, 0, h, :], in0=dprev[:], scalar=-slope,
                                                                             in1=mprev[:], op0=ALU.mult, op1=ALU.add),
                     reads=[Bb], writes=[Bb])
            P.dma(sp, es[:], sinks[l].partition_broadcast(128), writes=[Bb], join=True)
            P.op(act, lambda e: e.activation(out=es[:], in_=es[:], func=AF.Exp), reads=[Bb], writes=[Bb])
            qi = [sbt(st, f"cq{i}", [128, 8, 128], BF16) for i in range(2)]
            Bq = [Buf() for _ in range(2)]
            szs = [sbt(st, f"csz{i}", [128, 1024], BF16) for i in range(2)]
            br = [sbt(st, f"cbr{i}", [128, 1024], BF16) for i in range(2)]
            Bbr = [Buf() for _ in range(2)]
            brT = [sbt(st, f"cbrT{i}", [128, 8, 128], BF16) for i in range(2)]
            BbrT = [Buf() for _ in range(2)]
            rinv = [sbt(st, f"crinv{i}", [128, 1], F32) for i in range(2)]
            Brinv = [Buf() for _ in range(2)]
            tiles = []
            gi = 0
            for i in range(NB):
                ib = i % 2

                def load(i=i, ib=ib):
                    blk = slice(i * 128, (i + 1) * 128)
                    P.dma(sp, qi[ib][:], FM["QTc"][:, :, blk].rearrange("g p t -> p g t"), writes=[Bq[ib]])
                    P.dma(sp, szs[ib][:], SZ[blk, 2, :], writes=[Bq[ib]], join=True)

                tiles.append(dict(kind="call", fn=load))
                for h in range(16):
                    ob = O_BANKS[gi % 2]
                    gidx = gi % 2
                    gi += 1
                    g = h // 8
                    hp = (h % 2) * 64
                    js = [i] if i == 0 else [i - 1, i]
                    for j in js:
                        ks = slice(j * 128, (j + 1) * 128)
                        Sl = [(KTs[hp:hp + 64, g, ks], qi[ib][hp:hp + 64, h // 2, :], [Bres, Bq[ib]])]
                        tiles.append(dict(kind="tile", S=Sl, scale=scale, pre=BTc[:, 1 if j == i else 0, h, :], pre_reads=[Bb],
                                          pv_rhs=Vaug[:, j, g, :], pv_reads=[Bres], obank=ob, ow=65,
                                          start=(j == js[0]), stop=(j == i)))

                    def fin(ob=ob, h=h, ib=ib, gidx=gidx):
                        finalize(ob, 64, (es[:, h:h + 1], [Bb]), szs[ib][:, h * 64:(h + 1) * 64], Bq[ib],
                                 br[ib][:, h * 64:(h + 1) * 64], Bbr[ib], rinv[gidx], Brinv[gidx])

                    tiles.append(dict(kind="call", fn=fin))

                def fin_blk(i=i, ib=ib):
                    store_branch(st, 2, i, br[ib], Bbr[ib], brT[ib], BbrT[ib])

                tiles.append(dict(kind="call", fn=fin_blk))
            attn_stream(st, tiles)
        P.barrier()

    def mixer_A(l):
        scale = 128 ** -0.5
        rr["tb"] = (7,)
        with ExitStack() as st:
            VD = 128
            KTs = sbt(st, "aKT", [128, 2, S], BF16)
            IKs = sbt(st, "aIK", [128, S], BF16)
            Vaug = sbt(st, "aV", [128, NB, 2, 129], BF16)
            Bres = Buf()
            for g in range(2):
                P.dma(sp, KTs[:, g, :], FM["KTa"][g], writes=[Bres], join=(g > 0))
            P.dma(sp, IKs[:], FM["IKT"][0], writes=[Bres], join=True)
            Bv0 = Buf()
            for b0 in range(NB):
                P.op(pool, lambda e, b0=b0: e.memset(Vaug[:, b0, :, VD:VD + 1], 1.0), writes=[Bv0], inc=(b0 == NB - 1))
            for b0 in range(NB):
                P.dma(sp, Vaug[:, b0, :, 0:128],
                      Va[b0 * 128:(b0 + 1) * 128, :].rearrange("p (h c) -> p h c", c=128), reads=[Bv0], writes=[Bres], join=True)
            bi_i = sbt(st, "abi", [128, NB], I32)
            bi_f = sbt(st, "abf", [128, NB], F32)
            biasA = sbt(st, "abias", [128, 8, NB], F32)
            Bb = Buf()
            P.op(pool, lambda e: e.iota(bi_i[:], pattern=[[-128, NB]], base=0, channel_multiplier=1), writes=[Bb])
            P.op(dve, lambda e: e.tensor_copy(out=bi_f[:], in_=bi_i[:]), reads=[Bb], writes=[Bb])
            for h in range(8):
                slope = 2.0 ** (-8.0 * (h + 1) / 8)
                P.op(dve, lambda e, h=h, slope=slope: e.tensor_scalar(out=biasA[:, h, :], in0=bi_f[:], scalar1=slope,
                                                                      scalar2=None, op0=ALU.mult), reads=[Bb], writes=[Bb])
            qi = [sbt(st, f"aq{i}", [128, 8, 128], BF16) for i in range(2)]
            iq = [sbt(st, f"aiq{i}", [128, 8, 128], BF16) for i in range(2)]
            iw = [sbt(st, f"aiw{i}", [128, 16], F32) for i in range(2)]
            szs = [sbt(st, f"asz{i}", [128, 1024], BF16) for i in range(2)]
            Bq = [Buf() for _ in range(2)]
            Dg = sbt(st, "aDg", [128, 16, 128], BF16)
            BDg = Buf()
            rl = [sbt(st, f"arl{i}", [128, 512], BF16) for i in range(4)]
            Brl = [Buf() for _ in range(4)]
            score = sbt(st, "ascore", [128, S], F32)
            work = sbt(st, "awork", [128, S], F32)
            Bsc = Buf()
            Bwk = Buf()
            m8 = [sbt(st, f"am8{i}", [128, 8], F32) for i in range(2)]
            Bm8 = [Buf() for _ in range(2)]
            thr = sbt(st, "athr", [128, 1], F32)
            Bthr = Buf()
            kidx_i = sbt(st, "akidxi", [128, S], I32)
            kidx = sbt(st, "akidx", [128, S], F32)
            Bk = Buf()
            P.op(pool, lambda e: e.iota(kidx_i[:], pattern=[[1, S]], base=0, channel_multiplier=0), writes=[Bk])
            P.op(dve, lambda e: e.tensor_copy(out=kidx[:], in_=kidx_i[:]), reads=[Bk], writes=[Bk])
            smax = sbt(st, "asmax", [128, 1], F32)
            Bsm = Buf()
            Cd = [sbt(st, f"aCd{i}", [128, 8, 128], BF16) for i in range(2)]
            BCd = [Buf() for _ in range(2)]
            mb = [sbt(st, f"amb{i}", [128, S], BF16) for i in range(2)]
            Bmb = [Buf() for _ in range(2)]
            br = [sbt(st, f"abr{i}", [128, 1024], BF16) for i in range(2)]
            Bbr = [Buf() for _ in range(2)]
            brT = [sbt(st, f"abrT{i}", [128, 8, 128], BF16) for i in range(2)]
            BbrT = [Buf() for _ in range(2)]
            rinv = [sbt(st, f"arinv{i}", [128, 1], F32) for i in range(2)]
            Brinv = [Buf() for _ in range(2)]
            ZB = (4, 5)
            AB = 6
            tiles = []
            gi = 0
            zc = [0]

            def indexer(i, ib):
                blk = slice(i * 128, (i + 1) * 128)
                P.dma(sp, qi[ib][:], FM["QTa"][:, :, blk].rearrange("g p t -> p g t"), writes=[Bq[ib]])
                P.dma(sp, iq[ib][:], FM["IQT"][:, :, blk].rearrange("g p t -> p g t"), writes=[Bq[ib]], join=True)
                P.dma(sp, iw[ib][:], IW[blk, :], writes=[Bq[ib]], join=True)
                P.dma(sp, szs[ib][:], SZ[blk, 0, :], writes=[Bq[ib]], join=True)
                for h in range(16):
                    P.op(dve, lambda e, h=h: e.tensor_scalar(out=Dg[:, h, :], in0=ident[:], scalar1=iw[ib][:, h:h + 1],
                                                             scalar2=None, op0=ALU.mult), reads=[Bq[ib]], writes=[BDg],
                         inc=(h == 15))
                L = (i + 1) * 128
                for c0 in range(0, L, 512):
                    n = min(512, L - c0)
                    zs = []

                    def zmm(h):
                        zb = ZB[zc[0] % 2]
                        zc[0] += 1
                        hp = (h % 2) * 64
                        P.op(pe, lambda e: e.matmul(banks[zb][:, 0:n], lhsT=iq[ib][hp:hp + 64, h // 2, :],
                                                    rhs=IKs[hp:hp + 64, c0:c0 + n], start=True, stop=True),
                             reads=[Bq[ib], Bres], writes=[bankB[zb]])
                        r = rl[h % 4]
                        P.op(act, lambda e: e.activation(out=r[:, 0:n], in_=banks[zb][:, 0:n], func=AF.Relu),
                             reads=[bankB[zb]], writes=[Brl[h % 4]])

                    def amm(h):
                        P.op(pe, lambda e: e.matmul(banks[AB][:, 0:n], lhsT=Dg[:, h, :], rhs=rl[h % 4][:, 0:n],
                                                    start=(h == 0), stop=(h == 15)),
                             reads=[BDg, Brl[h % 4]], writes=[bankB[AB]], inc=(h == 15))

                    zmm(0)
                    for h in range(16):
                        if h + 1 < 16:
                            zmm(h + 1)
                        amm(h)
                    nfull = n - 128 if c0 + n == L else n
                    if nfull > 0:
                        P.op(dve, lambda e: e.tensor_copy(out=score[:, c0:c0 + nfull], in_=banks[AB][:, 0:nfull]),
                             reads=[bankB[AB]], writes=[Bsc])
                    if c0 + n == L:
                        P.op(dve, lambda e: e.tensor_tensor(out=score[:, L - 128:L], in0=banks[AB][:, n - 128:n], in1=causN[:],
                                                            op=ALU.add), reads=[bankB[AB], Bsc], writes=[Bsc])
                m = mb[ib]
                if L <= TOPK:
                    P.op(dve, lambda e: e.memset(thr[:], 0.5 * NEG), writes=[Bthr])
                if L > TOPK:
                    nit = TOPK // 8
                    for it in range(nit):
                        src = score if it == 0 else work
                        Bsrc = Bsc if it == 0 else Bwk
                        mm = m8[it % 2]
                        Bm = Bm8[it % 2]
                        P.op(dve, lambda e, src=src, mm=mm: e.max(out=mm[:], in_=src[:, 0:L]), reads=[Bsrc], writes=[Bm])
                        if it < nit - 1:
                            P.op(dve, lambda e, src=src, mm=mm: e.match_replace(out=work[:, 0:L], in_to_replace=mm[:],
                                                                                in_values=src[:, 0:L], imm_value=-3.0e38),
                                 reads=[Bsrc, Bm], writes=[Bwk])
                        else:
                            P.op(dve, lambda e, mm=mm: e.tensor_copy(out=thr[:], in_=mm[:, 7:8]), reads=[Bm], writes=[Bthr])
                P.op(dve, lambda e: e.tensor_scalar(out=m[:, 0:L], in0=score[:, 0:L], scalar1=thr[:, 0:1], scalar2=-1.0e9,
                                                    op0=ALU.is_lt, op1=ALU.mult), reads=[Bsc, Bthr], writes=[Bmb[ib]])
                P.op(dve, lambda e: e.scalar_tensor_tensor(out=work[:, 0:L], in0=score[:, 0:L], scalar=thr[:, 0:1], in1=kidx[:, 0:L],
                                                           op0=ALU.is_ge, op1=ALU.mult), reads=[Bsc, Bthr, Bk], writes=[Bwk])
                P.op(dve, lambda e: e.reduce_max(out=smax[:], in_=work[:, 0:L], axis=mybir.AxisListType.X), reads=[Bwk], writes=[Bsm])
                P.op(dve, lambda e: e.tensor_scalar(out=smax[:], in0=smax[:], scalar1=-1.0, scalar2=float(i * 128),
                                                    op0=ALU.mult, op1=ALU.add), reads=[Bsm], writes=[Bsm])
                for h in range(8):
                    slope = 2.0 ** (-8.0 * (h + 1) / 8)
                    P.op(dve, lambda e, h=h, slope=slope: e.tensor_scalar(out=Cd[ib][:, h, :], in0=ident[:], scalar1=smax[:, 0:1],
                                                                          scalar2=slope / scale, op0=ALU.mult, op1=ALU.mult),
                         reads=[Bsm], writes=[BCd[ib]], inc=(h == 7))

            for i in range(NB):
                ib = i % 2
                tiles.append(dict(kind="call", fn=lambda i=i, ib=ib: indexer(i, ib)))
                for h in range(8):
                    ob = O_BANKS[gi % 2]
                    gidx = gi % 2
                    gi += 1
                    g = h // 4
                    for j in range(i + 1):
                        ks = slice(j * 128, (j + 1) * 128)
                        Sl = [(KTs[:, g, ks], qi[ib][:, h, :], [Bres, Bq[ib]]),
                              (mb[ib][:, ks], ident[:], [Bmb[ib]]),
                              (ones_bf[:], Cd[ib][:, h, :], [BCd[ib]])]
                        tiles.append(dict(kind="tile", S=Sl, scale=scale, bias=biasA[:, h, (i - j):(i - j) + 1], bias_reads=[Bb],
                                          pv_rhs=Vaug[:, j, g, :], pv_reads=[Bres], obank=ob, ow=129,
                                          start=(j == 0), stop=(j == i)))

                    def fin(ob=ob, h=h, ib=ib, gidx=gidx):
                        finalize(ob, 128, None, szs[ib][:, h * 128:(h + 1) * 128], Bq[ib], br[ib][:, h * 128:(h + 1) * 128],
                                 Bbr[ib], rinv[gidx], Brinv[gidx])

                    tiles.append(dict(kind="call", fn=fin))

                def fin_blk(i=i, ib=ib):
                    store_branch(st, 0, i, br[ib], Bbr[ib], brT[ib], BbrT[ib])

                tiles.append(dict(kind="call", fn=fin_blk))
            attn_stream(st, tiles, sbanks=(0, 1))
        rr["tb"] = (6, 7)
        P.barrier()

    def stage_3a():
        with ExitStack() as st:
            xc = sbt(st, "3xc", [128, KC, 512], BF16)
            bc = sbt(st, "3bc", [128, 32, 512], BF16)
            Bx = Buf()
            wg = [sbt(st, f"3wg{i}", [128, KC, 512], BF16) for i in range(2)]
            wb = [sbt(st, f"3wb{i}", [128, 8, 512], BF16) for i in range(2)]
            Bw = [Buf() for _ in range(2)]
            acc = sbt(st, "3acc", [128, 4, 512], F32)
            Bacc = [Buf() for _ in range(4)]
            gs = [sbt(st, f"3gs{i}", [128, 512], F32) for i in range(2)]
            Bgs = [Buf() for _ in range(2)]
            mo = [sbt(st, f"3mo{i}", [128, 512], BF16) for i in range(2)]
            Bmo = [Buf() for _ in range(2)]
            wi = 0
            gk = 0
            for ch in range(NCH):
                t0 = ch * 512
                for k0 in range(0, KC, 8):
                    P.dma(sp, xc[:, k0:k0 + 8, :], XT[:, k0:k0 + 8, t0:t0 + 512], writes=[Bx], join=(k0 > 0))
                    P.dma(sp, bc[:, k0:k0 + 8, :], BT[:, k0:k0 + 8, t0:t0 + 512], writes=[Bx], join=True)
                for cgg in range(8):
                    for n in range(4):
                        w1 = wg[wi % 2]
                        w2 = wb[wi % 2]
                        Bw_ = Bw[wi % 2]
                        wi += 1
                        P.dma(sp, w1[:], WG[n * 8 + cgg], writes=[Bw_])
                        P.dma(sp, w2[:], WB[n * 8 + cgg], writes=[Bw_], join=True)
                        for sub in range(4):
                            bg = 2 + rr["bank"] % 6
                            rr["bank"] += 1
                            bp = 2 + rr["bank"] % 6
                            rr["bank"] += 1
                            for kc in range(KC):
                                P.op(pe, lambda e, kc=kc, bg=bg, sub=sub, w1=w1: e.matmul(
                                    banks[bg][:, :], lhsT=w1[:, kc, sub * 128:(sub + 1) * 128], rhs=xc[:, kc, :],
                                    start=(kc == 0), stop=(kc == KC - 1)), reads=[Bw_, Bx], writes=[bankB[bg]],
                                    inc=(kc == KC - 1))
                            for kc in range(8):
                                P.op(pe, lambda e, kc=kc, bp=bp, sub=sub, w2=w2, n=n: e.matmul(
                                    banks[bp][:, :], lhsT=w2[:, kc, sub * 128:(sub + 1) * 128], rhs=bc[:, n * 8 + kc, :],
                                    start=(kc == 0), stop=(kc == 7)), reads=[Bw_, Bx], writes=[bankB[bp]], inc=(kc == 7))
                            g_ = gs[gk % 2]
                            Bg_ = Bgs[gk % 2]
                            gk += 1
                            P.op(act, lambda e, g_=g_, bg=bg: e.activation(out=g_[:], in_=banks[bg][:, :], func=AF.Sigmoid),
                                 reads=[bankB[bg]], writes=[Bg_])
                            if n == 0:
                                P.op(dve, lambda e, g_=g_, bp=bp, sub=sub: e.tensor_tensor(out=acc[:, sub, :], in0=banks[bp][:, :],
                                                                                            in1=g_[:], op=ALU.mult),
                                     reads=[bankB[bp], Bg_], writes=[Bacc[sub]])
                            else:
                                P.op(dve, lambda e, g_=g_, bp=bp: e.tensor_tensor(out=g_[:], in0=banks[bp][:, :], in1=g_[:],
                                                                                  op=ALU.mult),
                                     reads=[bankB[bp], Bg_], writes=[Bg_])
                                if n < 3:
                                    P.op(pool, lambda e, g_=g_, sub=sub: e.tensor_tensor(out=acc[:, sub, :], in0=acc[:, sub, :],
                                                                                         in1=g_[:], op=ALU.add),
                                         reads=[Bg_, Bacc[sub]], writes=[Bacc[sub]])
                                else:
                                    o = mo[sub % 2]
                                    Bo = Bmo[sub % 2]
                                    P.op(pool, lambda e, g_=g_, sub=sub, o=o: e.tensor_tensor(out=o[:], in0=acc[:, sub, :],
                                                                                              in1=g_[:], op=ALU.add),
                                         reads=[Bg_, Bacc[sub]], writes=[Bo])
                                    P.dma(pool, MT[:, cgg * 4 + sub, t0:t0 + 512], o[:], reads=[Bo])
        P.barrier()

    def stage_3b(l, xsrc, last):
        TB = 2
        with ExitStack() as st:
            mc = sbt(st, "4mc", [128, KC, TB * 128], BF16)
            Bm = Buf()
            wo = [sbt(st, f"4wo{i}", [128, KC, 512], BF16) for i in range(2)]
            Bw = [Buf() for _ in range(2)]
            lg = sbt(st, "4lg", [128, D], F32)
            lb = sbt(st, "4lb", [128, D], F32)
            Bl = Buf()
            P.dma(sp, lg[:], ln_gain[l].partition_broadcast(128), writes=[Bl])
            P.dma(sp, lb[:], ln_bias[l].partition_broadcast(128), writes=[Bl], join=True)
            xr = [sbt(st, f"4xr{i}", [128, D], F32) for i in range(TB)]
            Bxr = [Buf() for _ in range(TB)]
            Bxq = [[Buf() for _ in range(8)] for _ in range(TB)]
            stats = sbt(st, "4st", [128, 8, 6], F32)
            mv = sbt(st, "4mv", [128, 2], F32)
            rstd = sbt(st, "4rstd", [128, 1], F32)
            nmr = sbt(st, "4nmr", [128, 1], F32)
            Bs = Buf()
            xb = [sbt(st, f"4xb{i}", [128, D], BF16) for i in range(2)]
            Bxb = [Buf() for _ in range(2)]
            xt = [sbt(st, f"4xt{i}", [128, KC, 128], BF16) for i in range(2)]
            Bxt = [[Buf() for _ in range(4)] for _ in range(2)]
            wi = 0
            for ch in range(S // (TB * 128)):
                t0 = ch * TB * 128
                for k0 in range(0, KC, 8):
                    P.dma(sp, mc[:, k0:k0 + 8, :], MT[:, k0:k0 + 8, t0:t0 + TB * 128], writes=[Bm], join=(k0 > 0))
                for tb in range(TB):
                    r0 = t0 + tb * 128
                    P.dma(sp, xr[tb][:], xsrc[r0:r0 + 128, :], writes=[Bxr[tb]] + Bxq[tb])
                for cgg in range(8):
                    w = wo[wi % 2]
                    Bw_ = Bw[wi % 2]
                    wi += 1
                    P.dma(sp, w[:], WO[cgg], writes=[Bw_])
                    cs_ = slice(cgg * 512, (cgg + 1) * 512)
                    for tb in range(TB):
                        bi = 2 + rr["bank"] % 4
                        rr["bank"] += 1
                        for kc in range(KC):
                            P.op(pe, lambda e, kc=kc, bi=bi, tb=tb, w=w: e.matmul(
                                banks[bi][:, :], lhsT=mc[:, kc, tb * 128:(tb + 1) * 128], rhs=w[:, kc, :],
                                start=(kc == 0), stop=(kc == KC - 1)), reads=[Bw_, Bm], writes=[bankB[bi]], inc=(kc == KC - 1))
                        P.op(dve, lambda e, bi=bi, tb=tb, cs_=cs_: e.scalar_tensor_tensor(
                            out=xr[tb][:, cs_], in0=xr[tb][:, cs_], scalar=ALPHA, in1=banks[bi][:, :], op0=ALU.mult, op1=ALU.add),
                            reads=[bankB[bi], Bxr[tb]], writes=[Bxq[tb][cgg]])
                for tb in range(TB):
                    r0 = t0 + tb * 128
                    X = xr[tb]
                    BX = [Bxr[tb]] + Bxq[tb]
                    for c in range(8):
                        P.op(dve, lambda e, c=c, X=X: e.bn_stats(out=stats[:, c, :], in_=X[:, c * 512:(c + 1) * 512]),
                             reads=BX, writes=[Bs], inc=(c == 7), join=(c > 0))
                    P.op(dve, lambda e: e.bn_aggr(out=mv[:], in_=stats[:]), reads=[Bs], writes=[Bs])
                    P.op(act, lambda e: e.activation(out=rstd[:], in_=mv[:, 1:2], func=AF.Sqrt, bias=epsln[:, 0:1], scale=1.0),
                         reads=[Bs], writes=[Bs])
                    P.op(dve, lambda e: e.reciprocal(out=rstd[:], in_=rstd[:]), reads=[Bs], writes=[Bs])
                    P.op(dve, lambda e: e.scalar_tensor_tensor(out=nmr[:], in0=mv[:, 0:1], scalar=-1.0, in1=rstd[:],
                                                               op0=ALU.mult, op1=ALU.mult), reads=[Bs], writes=[Bs])
                    P.op(act, lambda e, X=X: e.activation(out=X[:], in_=X[:], func=AF.Identity, bias=nmr[:, 0:1], scale=rstd[:, 0:1]),
                         reads=BX + [Bs], writes=BX)
                    P.op(dve, lambda e, X=X: e.tensor_tensor(out=X[:], in0=X[:], in1=lg[:], op=ALU.mult),
                         reads=BX + [Bl], writes=BX)
                    P.op(pool, lambda e, X=X: e.tensor_tensor(out=X[:], in0=X[:], in1=lb[:], op=ALU.add),
                         reads=BX + [Bl], writes=BX)
                    if last:
                        P.dma(pool, out_t[r0:r0 + 128, :], X[:], reads=BX)
                    else:
                        T3 = os.environ.get("T3", "abc")
                        if "a" in T3:
                            P.dma(pool, out_t[r0:r0 + 128, :], X[:], reads=BX)
                        i2 = tb % 2
                        if "b" in T3:
                            P.op(act, lambda e, X=X, i2=i2: e.copy(out=xb[i2][:], in_=X[:]), reads=BX, writes=[Bxb[i2]])
                        if "c" in T3:
                            transpose_block(xb[i2], KC, xt[i2], Bxt[i2], [Bxb[i2]])
                            for k0 in range(0, KC, 8):
                                P.dma(sp, XT[:, k0:k0 + 8, r0:r0 + 128], xt[i2][:, k0:k0 + 8, :], reads=[Bxt[i2][k0 // 8]])
        P.barrier()

    import os
    MIX = os.environ.get('MIX', 'ABCD')
    stage_R()
    for l in range(depth):
        steps = [lambda: (stage_W(l), P.barrier()),
                 lambda: stage_0(x_in) if l == 0 else None,
                 lambda: stage_1(),
                 lambda: stage_1b(l),
                 lambda: mixer_A(l) if "A" in MIX else None,
                 lambda: dense_mixer(l, "B") if "B" in MIX else None,
                 lambda: mixer_C(l) if "C" in MIX else None,
                 lambda: dense_mixer(l, "D") if "D" in MIX else None,
                 lambda: stage_3a(),
                 lambda: stage_3b(l, x_in if l == 0 else out_t, last=(l == depth - 1))]
        for si, fn in enumerate(steps):
            if int(os.environ.get('SKIPTO', '0')) <= l * 10 + si < upto:
                fn()
    if "MTD" in dbg:
        MTD = nc.dram_tensor("MTD", [128, KC, S], BF16, kind="ExternalOutput").ap()
        P.barrier()
        for k0 in range(0, 32, 8):
            P.dma(sp, MTD[:, k0:k0 + 8, :], MT[:, k0:k0 + 8, :])
    if "X1D" in dbg:
        X1D = nc.dram_tensor("X1D", [S, D], F32, kind="ExternalOutput").ap()
        P.barrier()
        for r0 in range(0, S, 512):
            P.dma(sp, X1D[r0:r0 + 512, :], X1[r0:r0 + 512, :])
    if "BTD" in dbg:
        BTD = nc.dram_tensor("BTD", [128, 32, S], BF16, kind="ExternalOutput").ap()
        P.barrier()
        for k0 in range(0, 32, 8):
            P.dma(sp, BTD[:, k0:k0 + 8, :], BT[:, k0:k0 + 8, :])
    P.barrier()
    nops = P.nops
    build_program.last_trace = P.trace
    P.close()
    gstack.close()
    return nc, nops


_CACHE = {}
FUSED = True


def _consts():
    cst = np.zeros((128, 4), np.float32)
    for p in range(128):
        cst[p, 0] = np.float32(ROPE_THETA) ** (-np.float32(p % 32) / np.float32(32))
        cst[p, 1] = -1.0 if (p % 64) < 32 else 1.0
    return cst


def kernel(x, positions, w_in, w_gate, w_branch, w_out, dq_gain, dq_up, dkv_gain, dkv_up, f_bias, sinks, ln_gain, ln_bias):
    B, S, _ = x.shape
    depth = w_in.shape[0]
    f = lambda a: np.ascontiguousarray(a, dtype=np.float32)
    weights = dict(w_in=w_in, w_gate=w_gate, w_branch=w_branch, w_out=w_out, dq_gain=dq_gain, dq_up=dq_up,
                   dkv_gain=dkv_gain, dkv_up=dkv_up, f_bias=f_bias, sinks=sinks, ln_gain=ln_gain, ln_bias=ln_bias)
    groups = [list(range(depth))] if FUSED else [[l] for l in range(depth)]
    cur = [f(x[b]) for b in range(B)]
    for g in groups:
        key = (S, len(g))
        if key not in _CACHE:
            _CACHE[key] = build_program(S, len(g))[0]
        nc = _CACHE[key]
        shared = {k: f(v[g[0]:g[-1] + 1]) for k, v in weights.items()}
        shared["consts"] = _consts()
        in_maps = []
        for b in range(B):
            m = dict(shared)
            m["x"] = cur[b]
            m["positions"] = np.ascontiguousarray(positions[b:b + 1], dtype=np.int32)
            in_maps.append(m)
        res = run_bass_kernel_spmd(nc, in_maps, core_ids=list(range(B)))
        cur = [np.ascontiguousarray(np.asarray(r["out"], dtype=np.float32)) for r in res.results]
    return np.stack(cur, axis=0)
```
